# Optimizing a Trainium2 kernel written in Bass

```python
import jax
import jax.numpy as jnp
from jax import lax
import numpy as np


D_MODEL = 1024
BATCH = 8
SEQ = 2048
DEPTH = 4

GRID_W = 64
CTX_LEN = 256
CONV_DIM = 512
CONV_WIDTH = 31
MLSTM_HEADS = 4
MLSTM_HEAD_DIM = 128
MLSTM_DIM = MLSTM_HEADS * MLSTM_HEAD_DIM
MIX_DIM = CONV_DIM + MLSTM_DIM
N_GATES = 2 * 2 * MLSTM_HEADS
Q_OFF = 2 * CONV_DIM
K_OFF = Q_OFF + MLSTM_DIM
V_OFF = K_OFF + MLSTM_DIM
O_OFF = V_OFF + MLSTM_DIM
G_OFF = O_OFF + MLSTM_DIM
IN_DIM = G_OFF + N_GATES
K_SCALE = MLSTM_HEAD_DIM ** -0.5
CHUNK = 64
D_FF = 2816
FFN_CONV_WIDTH = 3
N_MOD = 6
EPS = 1e-6

kernel_name = 'hybrid_conformer_mlstm_prefix_block'


def rms_norm(x, g):
    xf = x.astype(jnp.float32)
    y = xf * lax.rsqrt(jnp.mean(xf * xf, axis=-1, keepdims=True) + EPS)
    return (y * g.astype(jnp.float32)).astype(x.dtype)


def layer_norm(x, g, b):
    xf = x.astype(jnp.float32)
    mu = jnp.mean(xf, axis=-1, keepdims=True)
    var = jnp.mean(jnp.square(xf - mu), axis=-1, keepdims=True)
    return ((xf - mu) * lax.rsqrt(var + EPS) * g.astype(jnp.float32) + b.astype(jnp.float32)).astype(x.dtype)


def head_norm(h, g):
    hf = h.astype(jnp.float32)
    mu = jnp.mean(hf, axis=-1, keepdims=True)
    var = jnp.mean(jnp.square(hf - mu), axis=-1, keepdims=True)
    return (hf - mu) * lax.rsqrt(var + EPS) * g.reshape(MLSTM_HEADS, MLSTM_HEAD_DIM).astype(jnp.float32)


def modulate(h, shift, scale):
    return h * (1 + scale) + shift


def dwconv(x, w, b):
    k, ch = w.shape
    y = lax.conv_general_dilated(x, w[:, None, :].astype(x.dtype), window_strides=(1,),
                                 padding=[(k // 2, k // 2)],
                                 dimension_numbers=('NWC', 'WIO', 'NWC'),
                                 feature_group_count=ch)
    return y + b


def seq_conv(x, w, b, grid):
    bsz, length, ch = x.shape
    if grid:
        rows = length // GRID_W
        return dwconv(x.reshape(bsz * rows, GRID_W, ch), w, b).reshape(bsz, length, ch)
    return dwconv(x, w, b)


def _heads(t):
    bsz, length, _ = t.shape
    return t.reshape(bsz, length, MLSTM_HEADS, MLSTM_HEAD_DIM).transpose(0, 2, 1, 3)


def _gates(g, b):
    bsz, length, _ = g.shape
    return (g + b).astype(jnp.float32).reshape(bsz, length, 2, 2, MLSTM_HEADS).transpose(0, 2, 3, 4, 1)


def project(h, w, b_gates):
    a, gl, q, k, v, o, g = jnp.split(h @ w, [CONV_DIM, Q_OFF, K_OFF, V_OFF, O_OFF, G_OFF], axis=-1)
    return a, gl, _heads(q), _heads(k) * K_SCALE, _heads(v), o, _gates(g, b_gates)


def project_state_inputs(h, w, b_gates):
    k, v = jnp.split(h @ w[:, K_OFF:O_OFF], 2, axis=-1)
    g = h @ w[:, G_OFF:]
    return _heads(k) * K_SCALE, _heads(v), _gates(g, b_gates)


def zero_state(bsz):
    return (jnp.zeros((bsz, MLSTM_HEADS, MLSTM_HEAD_DIM, MLSTM_HEAD_DIM), jnp.float32),
            jnp.zeros((bsz, MLSTM_HEADS, MLSTM_HEAD_DIM), jnp.float32),
            jnp.zeros((bsz, MLSTM_HEADS), jnp.float32))


def chunk_gates(itil, logf):
    bsz, nh, length = itil.shape
    nc = length // CHUNK
    return itil.reshape(bsz, nh, nc, CHUNK), jnp.cumsum(logf.reshape(bsz, nh, nc, CHUNK), axis=-1)


def mlstm_states(kc, vc, ic, bcum, state):
    b_last = bcum[..., -1]
    a = b_last[..., None] - bcum + ic
    m_loc = jnp.max(a, axis=-1)
    w = jnp.exp(a - m_loc[..., None])
    c_loc = jnp.einsum('bhcl,bhclv,bhclk->bhcvk', w, vc, kc)
    n_loc = jnp.einsum('bhcl,bhclk->bhck', w, kc)

    def step(carry, inp):
        c_prev, n_prev, m_prev = carry
        c_l, n_l, m_l, b_l = inp
        m_new = jnp.maximum(b_l + m_prev, m_l)
        s_prev = jnp.exp(b_l + m_prev - m_new)
        s_loc = jnp.exp(m_l - m_new)
        c_new = s_prev[..., None, None] * c_prev + s_loc[..., None, None] * c_l
        n_new = s_prev[..., None] * n_prev + s_loc[..., None] * n_l
        return (c_new, n_new, m_new), (c_prev, n_prev, m_prev)

    xs = tuple(jnp.moveaxis(t, 2, 0) for t in (c_loc, n_loc, m_loc, b_last))
    final, entering = lax.scan(step, state, xs)
    entering = tuple(jnp.moveaxis(t, 0, 2) for t in entering)
    return entering, final


def mlstm_direction(q, k, v, itil, logf, state):
    bsz, nh, length, dh = q.shape
    nc = length // CHUNK
    qc, kc, vc = (t.reshape(bsz, nh, nc, CHUNK, dh) for t in (q, k, v))
    ic, bcum = chunk_gates(itil, logf)
    (c_in, n_in, m_in), final = mlstm_states(kc, vc, ic, bcum, state)
    order = jnp.arange(CHUNK)[:, None] >= jnp.arange(CHUNK)[None, :]
    d_log = jnp.where(order, bcum[..., :, None] - bcum[..., None, :] + ic[..., None, :], -jnp.inf)
    g_log = bcum + m_in[..., None]
    m_j = jnp.maximum(g_log, jnp.max(d_log, axis=-1))
    s = jnp.einsum('bhcjd,bhcsd->bhcjs', qc, kc) * jnp.exp(d_log - m_j[..., None])
    w_inter = jnp.exp(g_log - m_j)
    num = (jnp.einsum('bhcjs,bhcsd->bhcjd', s, vc)
           + w_inter[..., None] * jnp.einsum('bhcvk,bhcjk->bhcjv', c_in, qc))
    den = jnp.sum(s, axis=-1) + w_inter * jnp.einsum('bhck,bhcjk->bhcj', n_in, qc)
    h = num / jnp.maximum(jnp.abs(den), jnp.exp(-m_j))[..., None]
    return h.reshape(bsz, nh, length, dh), final


def _flip(t):
    return jnp.flip(t, axis=2)


def mlstm_bidir(q, k, v, gates, init):
    h_f, fin_f = mlstm_direction(q, k, v, gates[:, 0, 0], jax.nn.log_sigmoid(gates[:, 0, 1]), init[0])
    h_b, fin_b = mlstm_direction(_flip(q), _flip(k), _flip(v), _flip(gates[:, 1, 0]),
                                 _flip(jax.nn.log_sigmoid(gates[:, 1, 1])), init[1])
    return h_f + _flip(h_b), (fin_f, fin_b)


def mlstm_final_state(k, v, itil, logf, state):
    bsz, nh, length, dh = k.shape
    nc = length // CHUNK
    kc, vc = (t.reshape(bsz, nh, nc, CHUNK, dh) for t in (k, v))
    ic, bcum = chunk_gates(itil, logf)
    return mlstm_states(kc, vc, ic, bcum, state)[1]


def mlstm_bidir_final(k, v, gates, init):
    fin_f = mlstm_final_state(k, v, gates[:, 0, 0], jax.nn.log_sigmoid(gates[:, 0, 1]), init[0])
    fin_b = mlstm_final_state(_flip(k), _flip(v), _flip(gates[:, 1, 0]),
                              _flip(jax.nn.log_sigmoid(gates[:, 1, 1])), init[1])
    return (fin_f, fin_b)


def mixer_output(a, gl, o, hm, conv_w, conv_b, ln_g, ln_b, head_g, w_out, grid):
    u = a * jax.nn.sigmoid(gl)
    u = jax.nn.silu(layer_norm(seq_conv(u, conv_w, conv_b, grid), ln_g, ln_b))
    bsz, nh, length, dh = hm.shape
    m = jax.nn.sigmoid(o) * head_norm(hm.transpose(0, 2, 1, 3), head_g).reshape(bsz, length, MLSTM_DIM).astype(o.dtype)
    return jnp.concatenate([u, m.astype(u.dtype)], axis=-1) @ w_out


def conv_ffn(h, w_up, cw, cb, w_down, grid):
    val, gate = jnp.split(h @ w_up, 2, axis=-1)
    return (jax.nn.silu(seq_conv(gate, cw, cb, grid)) * val) @ w_down


def setup_inputs(seed: int = 0) -> dict:
    key = jax.random.key(seed)
    ks = jax.random.split(key, 20)

    def nrm(k, shape, std):
        return std * jax.random.normal(k, shape, jnp.float32)

    x = nrm(ks[0], (BATCH, SEQ, D_MODEL), 1.0)
    c = nrm(ks[1], (BATCH, D_MODEL), 1.0)
    ctx = nrm(ks[2], (BATCH, CTX_LEN, D_MODEL), 1.0)
    c_ctx = nrm(ks[3], (D_MODEL,), 1.0)
    w_ada = nrm(ks[4], (DEPTH, D_MODEL, N_MOD * D_MODEL), 0.5 * D_MODEL ** -0.5)
    b_ada = nrm(ks[5], (DEPTH, N_MOD * D_MODEL), 0.02)
    norm_g = 1.0 + nrm(ks[6], (DEPTH, 4, D_MODEL), 0.02)
    w_in = nrm(ks[7], (DEPTH, D_MODEL, IN_DIM), D_MODEL ** -0.5)
    f_bias = jnp.linspace(3.0, 6.0, MLSTM_HEADS, dtype=jnp.float32) + nrm(ks[8], (DEPTH, 2, 1, MLSTM_HEADS), 0.1)
    i_bias = nrm(ks[9], (DEPTH, 2, 1, MLSTM_HEADS), 0.1)
    b_gates = jnp.concatenate([i_bias, f_bias], axis=2).reshape(DEPTH, N_GATES)
    conv_w = nrm(ks[10], (DEPTH, CONV_WIDTH, CONV_DIM), CONV_WIDTH ** -0.5)
    conv_b = nrm(ks[11], (DEPTH, CONV_DIM), 0.02)
    conv_ln_g = 1.0 + nrm(ks[12], (DEPTH, CONV_DIM), 0.02)
    conv_ln_b = nrm(ks[13], (DEPTH, CONV_DIM), 0.02)
    mlstm_norm_g = 1.0 + nrm(ks[14], (DEPTH, MLSTM_DIM), 0.02)
    w_out = nrm(ks[15], (DEPTH, MIX_DIM, D_MODEL), MIX_DIM ** -0.5)
    w_up = nrm(ks[16], (DEPTH, D_MODEL, 2 * D_FF), D_MODEL ** -0.5)
    ffn_conv_w = nrm(ks[17], (DEPTH, FFN_CONV_WIDTH, D_FF), FFN_CONV_WIDTH ** -0.5)
    ffn_conv_b = nrm(ks[18], (DEPTH, D_FF), 0.02)
    w_down = nrm(ks[19], (DEPTH, D_FF, D_MODEL), D_FF ** -0.5)
    return {'x': x, 'c': c, 'ctx': ctx, 'c_ctx': c_ctx, 'w_ada': w_ada, 'b_ada': b_ada,
            'norm_g': norm_g, 'w_in': w_in, 'b_gates': b_gates, 'conv_w': conv_w,
            'conv_b': conv_b, 'conv_ln_g': conv_ln_g, 'conv_ln_b': conv_ln_b,
            'mlstm_norm_g': mlstm_norm_g, 'w_out': w_out, 'w_up': w_up,
            'ffn_conv_w': ffn_conv_w, 'ffn_conv_b': ffn_conv_b, 'w_down': w_down}


def reference(x, c, ctx, c_ctx, w_ada, b_ada, norm_g, w_in, b_gates, conv_w, conv_b,
              conv_ln_g, conv_ln_b, mlstm_norm_g, w_out, w_up, ffn_conv_w, ffn_conv_b, w_down):
    bsz = x.shape[0]
    s_x = jax.nn.silu(c)[:, None, :]
    s_c = jax.nn.silu(c_ctx)
    cs = ctx
    zero = zero_state(bsz)
    for l in range(DEPTH):
        last = l == DEPTH - 1
        mx = jnp.split(s_x @ w_ada[l] + b_ada[l], N_MOD, axis=-1)
        mc = jnp.split(s_c @ w_ada[l] + b_ada[l], N_MOD, axis=-1)
        hx = modulate(rms_norm(x, norm_g[l, 0]), mx[0], mx[1])
        hc = modulate(rms_norm(cs, norm_g[l, 0]), mc[0], mc[1])
        if last:
            kc, vc, gtc = project_state_inputs(hc, w_in[l], b_gates[l])
            fin_c = mlstm_bidir_final(kc, vc, gtc, (zero, zero))
        else:
            ac, glc, qc, kc, vc, oc, gtc = project(hc, w_in[l], b_gates[l])
            hm_c, fin_c = mlstm_bidir(qc, kc, vc, gtc, (zero, zero))
            yc = mixer_output(ac, glc, oc, hm_c, conv_w[l], conv_b[l], conv_ln_g[l], conv_ln_b[l],
                              mlstm_norm_g[l], w_out[l], False)
            cs = cs + mc[2] * rms_norm(yc, norm_g[l, 1])
            hc2 = modulate(rms_norm(cs, norm_g[l, 2]), mc[3], mc[4])
            cs = cs + mc[5] * rms_norm(conv_ffn(hc2, w_up[l], ffn_conv_w[l], ffn_conv_b[l], w_down[l], False),
                                       norm_g[l, 3])
        ax, glx, qx, kx, vx, ox, gtx = project(hx, w_in[l], b_gates[l])
        hm_x, _ = mlstm_bidir(qx, kx, vx, gtx, fin_c)
        yx = mixer_output(ax, glx, ox, hm_x, conv_w[l], conv_b[l], conv_ln_g[l], conv_ln_b[l],
                          mlstm_norm_g[l], w_out[l], True)
        x = x + mx[2] * rms_norm(yx, norm_g[l, 1])
        hx2 = modulate(rms_norm(x, norm_g[l, 2]), mx[3], mx[4])
        x = x + mx[5] * rms_norm(conv_ffn(hx2, w_up[l], ffn_conv_w[l], ffn_conv_b[l], w_down[l], True),
                                 norm_g[l, 3])
    return x
```

```python
import bisect
from contextlib import ExitStack

import numpy as np
import concourse.bass as bass
import concourse.mybir as mybir
from concourse.bass_utils import run_bass_kernel_spmd

F32 = mybir.dt.float32
BF16 = mybir.dt.bfloat16
AF = mybir.ActivationFunctionType
ALU = mybir.AluOpType
AX = mybir.AxisListType

D = 1024
NT = 2304
TCX = 256
DEPTH = 4
DFF = 2816
NFC = 22
EPS = 1e-6
KSCALE = 128 ** -0.5
_ESZ = {F32: 4, BF16: 2}


def _esz(dt):
    return _ESZ[dt]


def ap_intervals(ap):
    if ap.tensor.name.startswith("ps"):
        return [(0, 2048)]
    esz = _esz(ap.dtype)
    dims = list(ap.ap)
    pstride = dims[0][0]
    off = ap.offset
    free_off = off % pstride if pstride > 0 else off
    fd = [(abs(s), c) for (s, c) in dims[1:] if c > 1 and s != 0]
    if not fd:
        return [(free_off * esz, (free_off + 1) * esz)]
    fd.sort()
    s0, c0 = fd[0]
    span = (c0 - 1) * s0 + 1
    runs = [free_off]
    for (s, c) in fd[1:]:
        if len(runs) == 1 and s <= span:
            span = (c - 1) * s + span
        else:
            if len(runs) * c > 64:
                lo = runs[0]
                hi = runs[-1] + span + (c - 1) * s
                runs = [lo]
                span = hi - lo
            else:
                runs = [r + i * s for i in range(c) for r in runs]
                runs.sort()
    return [(r * esz, (r + span) * esz) for r in runs]


class _IMap:
    def __init__(self):
        self.starts = [0]
        self.segs = [[None, {}]]

    def _split(self, pos):
        i = bisect.bisect_right(self.starts, pos) - 1
        if self.starts[i] == pos:
            return i
        w, r = self.segs[i]
        self.starts.insert(i + 1, pos)
        self.segs.insert(i + 1, [w, dict(r)])
        return i + 1

    def rng(self, lo, hi):
        i = self._split(lo)
        j = self._split(hi)
        return range(i, j)


class Prog:
    ENGS = ("pe", "act", "dve", "pool", "sp")

    def __init__(self, nc, es):
        self.nc = nc
        self.es = es
        self.sem = {e: es.enter_context(nc.semaphore("s_" + e)) for e in ("pe", "act", "dve", "pool")}
        self.tick = {e: 0 for e in self.ENGS}
        self.ops = {e: [] for e in self.ENGS}
        self.seen = {e: {} for e in self.ENGS}
        self.dsem = []
        self.maps = {}
        self.nops = 0

    def dma_slot(self):
        s = self.es.enter_context(self.nc.semaphore("d%d" % len(self.dsem)))
        self.dsem.append([s, 0])
        return len(self.dsem) - 1

    def _segs(self, ap):
        m = self.maps.get(ap.tensor.name)
        if m is None:
            m = self.maps[ap.tensor.name] = _IMap()
        for lo, hi in ap_intervals(ap):
            for i in m.rng(lo, hi):
                yield m.segs[i]

    def emit(self, eng, fn, reads, writes, sig=True, dma=None):
        deps = {}

        def add(sv):
            if sv is None:
                return
            s, v = sv
            if s[0] == "d":
                v = self.dsem[s[1]][1]
            if deps.get(s, 0) < v:
                deps[s] = v

        rsegs = [sg for ap in reads for sg in self._segs(ap)]
        wsegs = [sg for ap in writes for sg in self._segs(ap)]
        for sg in rsegs:
            add(sg[0])
        for sg in wsegs:
            add(sg[0])
            for s, v in sg[1].items():
                add((s, v))
        if dma is not None:
            self.dsem[dma][1] += 16
            me = (("d", dma), self.dsem[dma][1])
        elif sig:
            self.tick[eng] += 1
            me = (("e", eng), self.tick[eng])
        else:
            me = (("e", eng), self.tick[eng] + 1)
        waits = []
        for s, v in deps.items():
            if s == ("e", "pe") and eng == "pe":
                continue
            if self.seen[eng].get(s, 0) >= v:
                continue
            self.seen[eng][s] = v
            waits.append((s, v))
        for sg in rsegs:
            if sg[1].get(me[0], 0) < me[1]:
                sg[1][me[0]] = me[1]
        for sg in wsegs:
            sg[0] = me
            sg[1] = {}
        self.ops[eng].append((fn, waits, (dma if dma is not None else (eng if sig else None))))
        self.nops += 1

    def mm(self, out, lhsT, rhs, start=True, stop=True):
        assert int(np.prod(out.shape[1:])) == int(np.prod(rhs.shape[1:])), (out.shape, rhs.shape)
        assert out.shape[0] == int(np.prod(lhsT.shape[1:])), (out.shape, lhsT.shape)
        assert lhsT.shape[0] == rhs.shape[0], (lhsT.shape, rhs.shape)
        self.emit("pe", lambda e: e.matmul(out, lhsT, rhs, start=start, stop=stop), [lhsT, rhs], [out], sig=stop)

    def tr(self, out, in_, ident):
        self.emit("pe", lambda e: e.transpose(out, in_, ident), [in_, ident], [out], sig=True)

    def act(self, out, in_, func, bias=None, scale=None):
        kw = {}
        rd = [in_]
        if bias is not None:
            kw["bias"] = bias
            if not isinstance(bias, (int, float)):
                rd.append(bias)
        if scale is not None:
            kw["scale"] = scale
            if not isinstance(scale, (int, float)):
                rd.append(scale)
        self.emit("act", lambda e: e.activation(out, in_, func, **kw), rd, [out])

    def tt(self, eng, out, in0, in1, op):
        self.emit(eng, lambda e: e.tensor_tensor(out, in0, in1, op), [in0, in1], [out])

    def ts(self, eng, out, in0, s1, op0, s2=None, op1=None):
        rd = [in0]
        if not isinstance(s1, (int, float)):
            rd.append(s1)
        if s2 is not None and not isinstance(s2, (int, float)):
            rd.append(s2)
        if op1 is None:
            self.emit(eng, lambda e: e.tensor_scalar(out, in0, s1, None, op0), rd, [out])
        else:
            self.emit(eng, lambda e: e.tensor_scalar(out, in0, s1, s2, op0, op1), rd, [out])

    def stt(self, out, in0, sc, in1, op0, op1):
        rd = [in0, in1]
        if not isinstance(sc, (int, float)):
            rd.append(sc)
        self.emit("dve", lambda e: e.scalar_tensor_tensor(out, in0, sc, in1, op0, op1), rd, [out])

    def copy(self, eng, out, in_):
        if eng == "act":
            self.emit("act", lambda e: e.copy(out, in_), [in_], [out])
        else:
            self.emit(eng, lambda e: e.tensor_copy(out, in_), [in_], [out])

    def memset(self, eng, out, val):
        self.emit(eng, lambda e: e.memset(out, val), [], [out])

    def recip(self, out, in_):
        self.emit("dve", lambda e: e.reciprocal(out, in_), [in_], [out])

    def load(self, out, in_, slot, q="sp"):
        self.emit(q, lambda e: e.dma_start(out=out, in_=in_), [], [out], dma=slot)

    def store(self, out, in_, slot, q="sp"):
        self.emit(q, lambda e: e.dma_start(out=out, in_=in_), [in_], [], dma=slot)

    def finalize(self):
        nc = self.nc
        fin = []
        for e in ("pe", "act", "dve", "pool"):
            if self.tick[e] > 0:
                fin.append((("e", e), self.tick[e]))
        for i, (s, c) in enumerate(self.dsem):
            if c > 0:
                fin.append((("d", i), c))
        self.ops["sp"].append((None, fin, None))

        def semof(s):
            return self.sem[s[1]] if s[0] == "e" else self.dsem[s[1]][0]

        def run(e, lst):
            for fn, waits, inc in lst:
                for s, v in waits:
                    e.wait_ge(semof(s), v)
                if fn is None:
                    continue
                ins = fn(e)
                if inc is None:
                    continue
                if isinstance(inc, int):
                    ins.then_inc(self.dsem[inc][0], 16)
                else:
                    ins.then_inc(self.sem[inc], 1)

        with nc.Block() as block:
            @block.tensor
            def _(e):
                run(e, self.ops["pe"])

            @block.scalar
            def _(e):
                run(e, self.ops["act"])

            @block.vector
            def _(e):
                run(e, self.ops["dve"])

            @block.gpsimd
            def _(e):
                run(e, self.ops["pool"])

            @block.sync
            def _(e):
                run(e, self.ops["sp"])


class Arena:
    def __init__(self, nc, es, name, nbytes):
        self.t = es.enter_context(nc.sbuf_tensor(name, [128, nbytes // 4], F32))
        self.ap = self.t[:, :] if not hasattr(self.t, "ap") else self.t.ap()
        self.cap = nbytes
        self.top = 0

    def at(self, off, shape, dt):
        n = int(np.prod(shape))
        nb = n * _esz(dt)
        assert off % 4 == 0 and off + nb <= self.cap, (off, nb, self.cap)
        a = self.ap[:, off // 4:(off + nb + 3) // 4]
        if dt != F32:
            a = a.bitcast(dt)[:, 0:n]
        if len(shape) == 2:
            a = a.rearrange("p (a b) -> p a b", b=shape[1])
        elif len(shape) == 3:
            a = a.rearrange("p (a b c) -> p a b c", b=shape[1], c=shape[2])
        elif len(shape) == 4:
            a = a.rearrange("p (a b c d) -> p a b c d", b=shape[1], c=shape[2], d=shape[3])
        return a

    def alloc(self, shape, dt):
        off = (self.top + 31) // 32 * 32
        n = int(np.prod(shape)) * _esz(dt)
        self.top = off + (n + 3) // 4 * 4
        assert self.top <= self.cap, ("arena overflow", self.top, self.cap)
        return self.at(off, shape, dt)


class Sub:
    def __init__(self, A, base, size):
        self.A, self.base, self.size, self.top = A, base, size, 0

    def reset(self):
        self.top = 0

    def alloc(self, shape, dt):
        off = (self.top + 31) // 32 * 32
        n = int(np.prod(shape)) * _esz(dt)
        self.top = off + (n + 3) // 4 * 4
        assert self.top <= self.size, ("sub overflow", self.top, self.size)
        return self.A.at(self.base + off, shape, dt)


V_NORMG, V_BADA, V_CONVW, V_CONVB, V_LNG, V_LNB, V_FFNW, V_FFNB = 0, 128, 320, 816, 832, 848, 864, 1128
V_IDENT, V_ONES, V_TRIF, V_TRIB, NVEC = 1216, 1344, 1472, 1600, 1728
NLV = 800
SUBG = [(i * 256, 256) for i in range(9)]
GROUPS = [(0, 256)] + [(256 + 512 * i, 512) for i in range(4)]
FBLOCKS = [[(0, 256), (256, 512), (768, 384)], [(1152, 512), (1664, 512), (2176, 128)]]
ORDER = [list(range(18)), [1, 0] + list(range(17, 1, -1))]


_DBG = {}


def build(depth=DEPTH, stop=None):
    nc = bass.Bass("TRN2", target_bir_lowering=False)
    xT = nc.dram_tensor("xT", [D, NT], F32, kind="ExternalInput").ap()
    cvec_d = nc.dram_tensor("cvec", [128, 16], F32, kind="ExternalInput").ap()
    vecs_d = nc.dram_tensor("vecs", [128, NVEC], F32, kind="ExternalInput").ap()
    lvecs_d = nc.dram_tensor("lvecs", [DEPTH, 128, NLV], F32, kind="ExternalInput").ap()
    w_ada = nc.dram_tensor("w_ada", [DEPTH, D, 6 * D], F32, kind="ExternalInput").ap()
    w_in = nc.dram_tensor("w_in", [DEPTH, D, 3088], F32, kind="ExternalInput").ap()
    w_out = nc.dram_tensor("wproj", [DEPTH, D, D], F32, kind="ExternalInput").ap()
    w_up = nc.dram_tensor("w_up", [DEPTH, D, 2 * DFF], F32, kind="ExternalInput").ap()
    w_down = nc.dram_tensor("w_down", [DEPTH, DFF, D], F32, kind="ExternalInput").ap()
    outT = nc.dram_tensor("outT", [D, NT], F32, kind="ExternalOutput").ap()

    with ExitStack() as es:
        P = Prog(nc, es)
        A = Arena(nc, es, "arena", 211968)
        psl = [es.enter_context(nc.psum_tensor("ps%d" % i, [128, 512], F32)) for i in range(6)]
        ps = [p[:, :] for p in psl]
        psbs = [es.enter_context(nc.psum_tensor("psb%d" % i, [128, 1024], BF16))[:, :] for i in range(2)]
        pctr = [0]

        def nps(lst=(0, 1, 2, 3, 4, 5)):
            pctr[0] += 1
            return ps[lst[pctr[0] % len(lst)]]

        bctr = [0]

        def npsb():
            bctr[0] += 1
            i = bctr[0] % 2
            return psbs[i][:, 0:128]

        X = A.alloc([8, NT], F32)
        V = A.alloc([NVEC], F32)
        LV = A.alloc([NLV], F32)
        CB = A.alloc([4, 128], BF16)
        identb, onesb, trifb, tribb = CB[:, 0, :], CB[:, 1, :], CB[:, 2, :], CB[:, 3, :]
        maskb = [trifb, tribb]
        identf, onesf = V[:, V_IDENT:V_IDENT + 128], V[:, V_ONES:V_ONES + 128]
        triff, tribf = V[:, V_TRIF:V_TRIF + 128], V[:, V_TRIB:V_TRIB + 128]
        cst = A.alloc([4], F32)
        c_eps, c_one = cst[:, 0:1], cst[:, 1:2]
        cv = A.alloc([8, 2], F32)
        sv = A.alloc([8, 2], F32)
        modv = A.alloc([DEPTH, 48, 2], F32)
        der = A.alloc([DEPTH * 4, 8, 2], F32)
        normg = V[:, V_NORMG:V_NORMG + 128].rearrange("p (l n k) -> p l n k", n=4, k=8)
        bada = V[:, V_BADA:V_BADA + 192].rearrange("p (l f) -> p l f", f=48)
        convw = V[:, V_CONVW:V_CONVW + 496].rearrange("p (l c k) -> p l c k", c=4, k=31)
        convb = V[:, V_CONVB:V_CONVB + 16].rearrange("p (l c) -> p l c", c=4)
        lng = V[:, V_LNG:V_LNG + 16].rearrange("p (l c) -> p l c", c=4)
        lnb = V[:, V_LNB:V_LNB + 16].rearrange("p (l c) -> p l c", c=4)
        ffnw = V[:, V_FFNW:V_FFNW + 264].rearrange("p (l f k) -> p l f k", f=22, k=3)
        ffnb = V[:, V_FFNB:V_FFNB + 88].rearrange("p (l f) -> p l f", f=22)
        mng = LV[:, 0:512]
        bgi = LV[:, 512:656].rearrange("p (t d h) -> p t d h", d=2, h=4)
        bgf = LV[:, 656:800].rearrange("p (t d h) -> p t d h", d=2, h=4)
        WST = [A.alloc([2048], F32) for _ in range(2)]
        WBF = [A.alloc([2048], BF16) for _ in range(4)]
        wst_sem = [P.dma_slot() for _ in range(2)]
        wctr = [0, 0]
        H_off = (A.top + 31) // 32 * 32
        H = A.alloc([8, NT], BF16)
        QM_off = (A.top + 31) // 32 * 32
        QM = A.alloc([4, NT], BF16)
        S_off = (A.top + 31) // 32 * 32
        S = Sub(A, S_off, A.cap - S_off)
        HS = Sub(A, H_off + 8 * 1152 * 2, 8 * 1152 * 2)
        G = A.at(QM_off, [NFC, 1152], BF16)
        H2 = A.at(H_off, [8, 1152], BF16)
        sem_x, sem_v, sem_lv, sem_o = P.dma_slot(), P.dma_slot(), P.dma_slot(), P.dma_slot()
        _DBG.update(H_off=H_off, QM_off=QM_off, S_off=S_off)

        def wload(pieces, nk):
            si = wctr[0] % 2
            wctr[0] += 1
            bi = wctr[1] % 4
            wctr[1] += 1
            off = 0
            outs = []
            for pc in pieces:
                ncol = pc.shape[2]
                n = nk * ncol
                stv = WST[si][:, off:off + n].rearrange("p (k c) -> p k c", c=ncol)
                P.load(stv, pc, wst_sem[si])
                outs.append(WBF[bi][:, off:off + n].rearrange("p (k c) -> p k c", c=ncol))
                off += n
            assert off <= 2048
            P.copy("pool", WBF[bi][:, 0:off], WST[si][:, 0:off])
            return outs

        def wcols(w3, l, c0, ncol):
            return w3[l].rearrange("(k p) c -> p k c", p=128)[:, :, c0:c0 + ncol]

        for k in range(8):
            P.load(X[:, k, :], xT[k * 128:(k + 1) * 128, :], sem_x)
        P.load(V, vecs_d, sem_v)
        P.load(cv, cvec_d.rearrange("p (k w) -> p k w", w=2), sem_v)
        P.memset("dve", cst[:, 0:1], EPS)
        P.memset("dve", cst[:, 1:2], 1.0)
        P.copy("dve", CB[:, 0, :], identf)
        P.copy("dve", CB[:, 1, :], onesf)
        P.copy("dve", CB[:, 2, :], triff)
        P.copy("dve", CB[:, 3, :], tribf)
        P.act(sv, cv, AF.Silu)
        for l in range(depth):
            pm = nps()
            for j in range(24):
                si = wctr[0] % 2
                wctr[0] += 1
                stv = WST[si].rearrange("p (k c) -> p k c", c=256)
                P.load(stv, wcols(w_ada, l, j * 256, 256), wst_sem[si])
                for fc in range(2):
                    f = j * 2 + fc
                    for k in range(8):
                        P.mm(pm[:, 2 * f:2 * f + 2], stv[:, k, fc * 128:(fc + 1) * 128], sv[:, k, :],
                             start=(k == 0), stop=(k == 7))
            pm3 = pm[:, 0:96].rearrange("p (f w) -> p f w", w=2)
            for w in range(2):
                P.tt("dve", modv[:, l, :, w], pm3[:, :, w], bada[:, l, :], ALU.add)
            for w in range(2):
                P.stt(der[:, l * 4 + 0, :, w], modv[:, l, 8:16, w], 1.0, normg[:, l, 0, :], ALU.add, ALU.mult)
                P.tt("dve", der[:, l * 4 + 1, :, w], modv[:, l, 16:24, w], normg[:, l, 1, :], ALU.mult)
                P.stt(der[:, l * 4 + 2, :, w], modv[:, l, 32:40, w], 1.0, normg[:, l, 2, :], ALU.add, ALU.mult)
                P.tt("dve", der[:, l * 4 + 3, :, w], modv[:, l, 40:48, w], normg[:, l, 3, :], ALU.mult)

        def split3(src, spl, spr, n, rows_=128):
            h = [spl[0:rows_, j, 0:n] for j in range(3)]
            r = spr[0:rows_, 0:n]
            P.copy("dve", h[0], src)
            P.tt("dve", r, src, h[0], ALU.subtract)
            P.copy("dve", h[1], r)
            P.tt("dve", r, r, h[1], ALU.subtract)
            P.copy("dve", h[2], r)
            return h

        def rstd_from(psum_ap, out_ap, inv_n):
            P.act(out_ap, psum_ap, AF.Ln, bias=c_eps, scale=inv_n)
            P.act(out_ap, out_ap, AF.Exp, scale=-0.5)

        def normmod(l, subs, kind_gs, sh_base, dst, sub):
            sq = [sub.alloc([8, 256], BF16) for _ in range(2)]
            rsb = [sub.alloc([256], F32) for _ in range(2)]
            tmp = [sub.alloc([256], F32) for _ in range(3)]
            for i, (t0, n) in enumerate(subs):
                w = 1 if t0 < TCX else 0
                pp = nps()
                for k in range(8):
                    P.tt("pool", sq[i % 2][:, k, 0:n], X[:, k, t0:t0 + n], X[:, k, t0:t0 + n], ALU.mult)
                for k in range(8):
                    P.mm(pp[:, 0:n], onesb, sq[i % 2][:, k, 0:n], start=(k == 0), stop=(k == 7))
                rstd_from(pp[:, 0:n], rsb[i % 2][:, 0:n], 1.0 / D)
                for k in range(8):
                    tt_ = tmp[k % 3][:, 0:n]
                    P.tt("dve", tt_, X[:, k, t0:t0 + n], rsb[i % 2][:, 0:n], ALU.mult)
                    P.act(dst(k, t0, n), tt_, AF.Identity, bias=modv[:, l, sh_base + k, w:w + 1],
                          scale=der[:, l * 4 + kind_gs, k, w:w + 1])

        def layer(l):
            P.load(LV, lvecs_d[l], sem_lv)
            if stop == 'setup':
                return
            S.reset()
            normmod(l, SUBG, 0, 0, lambda k, t0, n: H[:, k, t0:t0 + n], S)
            if stop == 'norm1':
                return
            S.reset()
            Kb = S.alloc([NT], BF16)
            VTM = S.alloc([18, 130], BF16)
            hs_off = S.base + (S.top + 31) // 32 * 32
            HSUM = S.alloc([18, 128], F32)
            GI = S.alloc([18, 2, 4], F32)
            LF = S.alloc([18, 2, 4], F32)
            NB = S.alloc([18, 2, 4], F32)
            DD = S.alloc([18, 2, 4], F32)
            rows = S.alloc([4, 144], F32)
            MB = S.alloc([288], F32)
            T3 = S.alloc([3, 144], F32)
            SPL = A.at(hs_off, [3, 288], BF16)
            SPR = A.at(hs_off + 2048, [288], F32)
            E3 = S.alloc([3, 144], F32)
            UG = E3[:, 0, :].rearrange("p (t d h) -> p t d h", d=2, h=4)
            WB = E3[:, 1, :].rearrange("p (t d h) -> p t d h", d=2, h=4)
            CL = E3[:, 2, :].rearrange("p (t d h) -> p t d h", d=2, h=4)
            Cst = [S.alloc([129], F32) for _ in range(2)]
            Csb = [[S.alloc([130], BF16) for _ in range(2)] for _ in range(2)]
            PT = [[S.alloc([128], BF16) for _ in range(2)] for _ in range(2)]
            KU = [[S.alloc([128], BF16) for _ in range(2)] for _ in range(2)]
            dn = S.alloc([8], F32)
            hdt = [S.alloc([128], F32) for _ in range(2)]
            stt_ = S.alloc([18, 6], F32)
            mv = S.alloc([18, 2], F32)
            rsd = S.alloc([18], F32)
            OG = [S.alloc([128], F32) for _ in range(2)]
            XN = hdt
            MT = [S.alloc([128], BF16) for _ in range(2)]

            (wg,) = wload([wcols(w_in, l, 3072, 16)], 8)
            pg = nps()
            for t in range(18):
                for k in range(8):
                    P.mm(pg[:, t * 16:(t + 1) * 16], H[:, k, t * 128:(t + 1) * 128], wg[:, k, :],
                         start=(k == 0), stop=(k == 7))
            pg5 = pg[:, 0:288].rearrange("p (t d g h) -> p t d g h", d=2, g=2, h=4)
            P.tt("dve", GI, pg5[:, :, :, 0, :], bgi, ALU.add)
            P.tt("dve", LF, pg5[:, :, :, 1, :], bgf, ALU.add)
            if stop == 'g1':
                return
            LFf = LF.rearrange("p t d h -> p (t d h)")
            GIf = GI.rearrange("p t d h -> p (t d h)")
            NBf = NB.rearrange("p t d h -> p (t d h)")
            DDf = DD.rearrange("p t d h -> p (t d h)")
            P.ts("dve", LFf, LFf, -60.0, ALU.max)
            P.act(LFf, LFf, AF.Exp, scale=-1.0)
            P.act(LFf, LFf, AF.Ln, bias=c_one, scale=1.0)
            if stop == 'g2a':
                return
            pc_ = nps()
            L3 = split3(LFf, SPL, SPR, 144)
            for j3, hb in enumerate(L3):
                P.mm(pc_[:, 0:144], trifb, hb, start=(j3 == 0), stop=(j3 == 2))
            pc2 = nps()
            for j3, hb in enumerate(L3):
                P.mm(pc2[:, 0:144], tribb, hb, start=(j3 == 0), stop=(j3 == 2))
            pc3 = nps()
            for j3, hb in enumerate(L3):
                P.mm(pc3[:, 0:144], onesb, hb, start=(j3 == 0), stop=(j3 == 2))
            pcf = pc_[:, 0:144].rearrange("p (t d h) -> p t d h", d=2, h=4)
            pcb = pc2[:, 0:144].rearrange("p (t d h) -> p t d h", d=2, h=4)
            P.copy("act", NB[:, :, 0, :], pcf[:, :, 0, :])
            P.copy("act", NB[:, :, 1, :], pcb[:, :, 1, :])
            P.tt("dve", DDf, GIf, NBf, ALU.add)
            if stop == 'g2':
                return
            P.emit("pool", lambda e: e.tensor_reduce(rows[0:1, 0, :], DDf, AX.C, ALU.max), [DDf], [rows[0:1, 0, :]])
            P.copy("act", rows[0:1, 1, :], pc3[0:1, 0:144])
            P.memset("dve", rows[0:1, 3, :], 0.0)
            if stop == 'g3':
                return
            r4 = rows[0:1, :, :].rearrange("p r (t d h) -> p r t d h", d=2, h=4)
            for dr in range(2):
                od = ORDER[dr]
                for i, t in enumerate(od):
                    P.tt("dve", r4[:, 2, t, dr, :], r4[:, 3, t, dr, :], r4[:, 0, t, dr, :], ALU.max)
                    if i + 1 < 18:
                        P.tt("dve", r4[:, 3, od[i + 1], dr, :], r4[:, 2, t, dr, :], r4[:, 1, t, dr, :], ALU.subtract)
            if stop == 'g4':
                return
            pmb = nps()
            R3 = split3(rows[0:1, 2:4, :].rearrange("p r n -> p (r n)"), SPL, SPR, 288, rows_=1)
            for j3, hb in enumerate(R3):
                P.mm(pmb[:, 0:288], onesb[0:1, :], hb, start=(j3 == 0), stop=(j3 == 2))
            P.copy("act", MB, pmb[:, 0:288])
            P.tt("dve", T3[:, 0, :], DDf, MB[:, 0:144], ALU.subtract)
            P.tt("dve", T3[:, 1, :], MB[:, 144:288], MB[:, 0:144], ALU.subtract)
            P.tt("dve", T3[:, 2, :], NBf, MB[:, 0:144], ALU.subtract)
            P.ts("dve", T3[:, 2, :], T3[:, 2, :], 80.0, ALU.min)
            P.act(E3.rearrange("p a b -> p (a b)"), T3.rearrange("p a b -> p (a b)"), AF.Exp)
            P.memset("pool", VTM[:, :, 128:130], 1.0)

            if stop == 'gates':
                return
            for hd in range(4):
                wq, wk = wload([wcols(w_in, l, 1024 + hd * 128, 128), wcols(w_in, l, 1536 + hd * 128, 128)], 8)
                wv, wo = wload([wcols(w_in, l, 2048 + hd * 128, 128), wcols(w_in, l, 2560 + hd * 128, 128)], 8)
                for (t0, n) in GROUPS:
                    pq = nps()
                    for k in range(8):
                        P.mm(pq[:, 0:n], wq[:, k, :], H[:, k, t0:t0 + n], start=(k == 0), stop=(k == 7))
                    P.act(QM[:, hd, t0:t0 + n], pq[:, 0:n], AF.Copy, scale=KSCALE)
                    pk = nps()
                    for k in range(8):
                        P.mm(pk[:, 0:n], wk[:, k, :], H[:, k, t0:t0 + n], start=(k == 0), stop=(k == 7))
                    P.copy("dve", Kb[:, t0:t0 + n], pk[:, 0:n])
                for t4 in range(0, 18, 4):
                    nt_ = min(4, 18 - t4)
                    pv = nps()
                    for i in range(nt_):
                        t = t4 + i
                        for k in range(8):
                            P.mm(pv[:, i * 128:(i + 1) * 128], H[:, k, t * 128:(t + 1) * 128], wv[:, k, :],
                                 start=(k == 0), stop=(k == 7))
                    P.copy("act", VTM[:, t4:t4 + nt_, 0:128],
                           pv[:, 0:nt_ * 128].rearrange("p (a b) -> p a b", b=128))
                P.memset("pool", HSUM, 0.0)
                for dr in range(2):
                    P.memset("pool", Cst[dr], 0.0)
                for step in range(18):
                    for dr in range(2):
                        t = ORDER[dr][step]
                        par = step % 2
                        tk = slice(t * 128, (t + 1) * 128)
                        u_ap = UG[:, t, dr, hd:hd + 1]
                        w_ap = WB[:, t, dr, hd:hd + 1]
                        pS = nps()
                        P.mm(pS[:, 0:128], Kb[:, tk], QM[:, hd, tk])
                        pT = npsb()
                        P.tr(pT, Kb[:, tk], identb)
                        P.stt(PT[dr][par], pS[:, 0:128], u_ap, maskb[dr], ALU.mult, ALU.mult)
                        P.act(KU[dr][par], pT, AF.Copy, scale=u_ap)
                        P.ts("pool", Csb[dr][par][:, 0:129], Cst[dr], w_ap, ALU.mult)
                        pN = nps()
                        P.mm(pN[:, 0:129], PT[dr][par], VTM[:, t, 0:129], start=True, stop=False)
                        P.mm(pN[:, 0:129], QM[:, hd, tk], Csb[dr][par][:, 0:129], start=False, stop=True)
                        pC = nps()
                        P.mm(pC[:, 0:129], KU[dr][par], VTM[:, t, 0:129])
                        P.stt(Cst[dr], Cst[dr], w_ap, pC[:, 0:129], ALU.mult, ALU.add)
                        dcol = dn[:, dr * 2:dr * 2 + 1]
                        rcol = dn[:, dr * 2 + 1:dr * 2 + 2]
                        P.ts("dve", dcol, pN[:, 128:129], -1.0, ALU.mult, CL[:, t, dr, hd:hd + 1], ALU.max)
                        P.ts("dve", dcol, pN[:, 128:129], dcol, ALU.max)
                        P.recip(rcol, dcol)
                        P.act(hdt[dr], pN[:, 0:128], AF.Copy, scale=rcol)
                        P.tt("pool", HSUM[:, t, :], HSUM[:, t, :], hdt[dr], ALU.add)
                for t in range(18):
                    P.emit("dve", lambda e, t=t: e.bn_stats(stt_[:, t, :], HSUM[:, t, :]), [HSUM[:, t, :]], [stt_[:, t, :]])
                    P.emit("dve", lambda e, t=t: e.bn_aggr(mv[:, t, :], stt_[:, t, :]), [stt_[:, t, :]], [mv[:, t, :]])
                P.act(rsd, mv[:, :, 1], AF.Ln, bias=c_eps, scale=1.0)
                P.act(rsd, rsd, AF.Exp, scale=-0.5)
                for t4 in range(0, 18, 4):
                    nt_ = min(4, 18 - t4)
                    pbs = []
                    for i in range(nt_):
                        t = t4 + i
                        po = nps()
                        for k in range(8):
                            P.mm(po[:, 0:128], H[:, k, t * 128:(t + 1) * 128], wo[:, k, :], start=(k == 0), stop=(k == 7))
                        P.act(OG[t % 2], po[:, 0:128], AF.Sigmoid)
                        P.tt("pool", OG[t % 2], OG[t % 2], mng[:, hd * 128:(hd + 1) * 128], ALU.mult)
                        P.ts("dve", XN[t % 2], HSUM[:, t, :], mv[:, t, 0:1], ALU.subtract, rsd[:, t:t + 1], ALU.mult)
                        P.tt("dve", MT[t % 2], XN[t % 2], OG[t % 2], ALU.mult)
                        pb = npsb()
                        P.tr(pb, MT[t % 2], identb)
                        P.copy("act", QM[:, hd, t * 128:(t + 1) * 128], pb)

            if stop == 'mlstm':
                return
            S.reset()
            UB = S.alloc([4, NT], BF16)
            s_mark = S.top
            UP = S.alloc([2880], BF16)
            SG = [S.alloc([512], F32) for _ in range(2)]
            P.memset("pool", UP, 0.0)

            def up_view(t0, n, shift):
                if t0 < TCX:
                    return UP[:, 16 + shift:16 + shift + 256]
                r0 = (t0 - TCX) // 64
                base = 288 + 80 * r0 + shift
                rws = n // 64
                return UP[:, base:base + 80 * rws].rearrange("p (r c) -> p r c", c=80)[:, :, 0:64]

            def rows_view(ap2, t0, n):
                if t0 < TCX:
                    return ap2
                return ap2.rearrange("p (r c) -> p r c", c=64)

            for c in range(4):
                wa, wgl = wload([wcols(w_in, l, c * 128, 128), wcols(w_in, l, 512 + c * 128, 128)], 8)
                if c == 0:
                    DBt = S.alloc([31, 128], BF16)
                for kk in range(31):
                    P.ts("pool" if kk % 2 else "dve", DBt[:, kk, :], identb, convw[:, l, c, kk:kk + 1], ALU.mult)
                for gi, (t0, n) in enumerate(GROUPS):
                    pa = nps()
                    for k in range(8):
                        P.mm(pa[:, 0:n], wa[:, k, :], H[:, k, t0:t0 + n], start=(k == 0), stop=(k == 7))
                    pgl = nps()
                    for k in range(8):
                        P.mm(pgl[:, 0:n], wgl[:, k, :], H[:, k, t0:t0 + n], start=(k == 0), stop=(k == 7))
                    P.act(SG[gi % 2][:, 0:n], pgl[:, 0:n], AF.Sigmoid)
                    P.tt("dve", up_view(t0, n, 0), rows_view(pa[:, 0:n], t0, n), rows_view(SG[gi % 2][:, 0:n], t0, n), ALU.mult)
                    py = nps()
                    for kk in range(31):
                        P.mm(rows_view(py[:, 0:n], t0, n), DBt[:, kk, :], up_view(t0, n, kk - 15),
                             start=(kk == 0), stop=(kk == 30))
                    P.act(UB[:, c, t0:t0 + n], py[:, 0:n], AF.Identity, bias=convb[:, l, c:c + 1], scale=1.0)
            if stop == 'c1':
                return
            S.top = s_mark
            SQ = [S.alloc([4, 256], BF16) for _ in range(2)]
            MU = [S.alloc([256], F32) for _ in range(2)]
            RS = [S.alloc([256], F32) for _ in range(2)]
            TM = [S.alloc([256], F32) for _ in range(2)]
            for i, (t0, n) in enumerate(SUBG):
                p1, p2 = nps(), nps()
                for c in range(4):
                    P.act(SQ[i % 2][:, c, :], UB[:, c, t0:t0 + n], AF.Square)
                for c in range(4):
                    P.mm(p1[:, 0:n], onesb, UB[:, c, t0:t0 + n], start=(c == 0), stop=(c == 3))
                for c in range(4):
                    P.mm(p2[:, 0:n], onesb, SQ[i % 2][:, c, :], start=(c == 0), stop=(c == 3))
                mu, rs = MU[i % 2], RS[i % 2]
                P.act(mu, p1[:, 0:n], AF.Copy, scale=1.0 / 512)
                P.tt("dve", rs, mu, mu, ALU.mult)
                P.stt(rs, p2[:, 0:n], 1.0 / 512, rs, ALU.mult, ALU.subtract)
                P.ts("dve", rs, rs, 0.0, ALU.max)
                P.act(rs, rs, AF.Ln, bias=c_eps, scale=1.0)
                P.act(rs, rs, AF.Exp, scale=-0.5)
                for c in range(4):
                    tm = TM[c % 2]
                    P.tt("dve", tm, UB[:, c, t0:t0 + n], mu, ALU.subtract)
                    P.tt("pool", tm, tm, rs, ALU.mult)
                    P.act(UB[:, c, t0:t0 + n], tm, AF.Silu, bias=lnb[:, l, c:c + 1], scale=lng[:, l, c:c + 1])

            if stop == 'conv':
                return
            wo4 = []
            for i in range(4):
                (wv_,) = wload([wcols(w_out, l, i * 256, 256)], 8)
                wo4.append(wv_)
            S.top = s_mark
            YS = [S.alloc([8, 256], BF16) for _ in range(1)]
            YF = S.alloc([8, 256], F32)
            RS2 = [S.alloc([256], F32) for _ in range(2)]
            TM2 = [S.alloc([256], F32) for _ in range(2)]
            for i, (t0, n) in enumerate(SUBG):
                w = 1 if t0 < TCX else 0
                for dh in range(2):
                    for d in range(4 * dh, 4 * dh + 4):
                        yo = ps[d % 4][:, 0:n]
                        for k in range(8):
                            rhs = UB[:, k, t0:t0 + n] if k < 4 else QM[:, k - 4, t0:t0 + n]
                            P.mm(yo, wo4[d // 2][:, k, (d % 2) * 128:(d % 2) * 128 + 128], rhs, start=(k == 0), stop=(k == 7))
                    for d in range(4 * dh, 4 * dh + 4):
                        yo = ps[d % 4][:, 0:n]
                        P.act(YF[:, d, :], yo, AF.Copy, scale=1.0)
                        P.tt("pool", YS[0][:, d, :], YF[:, d, :], YF[:, d, :], ALU.mult)
                pst = ps[4 + i % 2]
                for d in range(8):
                    P.mm(pst[:, 0:n], onesb, YS[0][:, d, :], start=(d == 0), stop=(d == 7))
                if stop == 'w2':
                    continue
                rstd_from(pst[:, 0:n], RS2[i % 2], 1.0 / D)
                for d in range(8):
                    P.tt("dve", TM2[d % 2], YF[:, d, :], RS2[i % 2], ALU.mult)
                    if stop != 'w3':
                        P.stt(X[:, d, t0:t0 + n], TM2[d % 2], der[:, l * 4 + 1, d, w:w + 1], X[:, d, t0:t0 + n], ALU.mult, ALU.add)

            if stop == 'wout':
                return
            for blk in FBLOCKS:
                b0 = blk[0][0]
                HS.reset()
                subs = [(b0 + i * 256, 256) for i in range(1152 // 256)] + [(b0 + 1024, 128)]
                normmod(l, subs, 2, 24, lambda k, t0, n: H2[:, k, t0 - b0:t0 - b0 + n], HS)
                if stop == 'f1':
                    return
                HS.reset()
                GP = [HS.alloc([528], F32) for _ in range(2)]
                ACC = [HS.alloc([512], F32) for _ in range(2)]
                SL = [HS.alloc([512], F32) for _ in range(2)]
                GPC = HS.alloc([264], F32)
                for gp in GP + [GPC]:
                    P.memset("pool", gp, 0.0)

                def gp_view(gp, t0, n, sh):
                    if t0 < TCX:
                        return gp[:, sh:sh + 256]
                    rws = n // 64
                    return gp[:, 0:66 * rws].rearrange("p (r c) -> p r c", c=66)[:, :, sh:sh + 64]

                cnt = 0
                for fp in range(11):
                    (wvl,) = wload([wcols(w_up, l, fp * 256, 256)], 8)
                    (wgt,) = wload([wcols(w_up, l, DFF + fp * 256, 256)], 8)
                    for fc in range(2):
                        f = fp * 2 + fc
                        for (t0, n) in blk:
                            pvv = nps()
                            for k in range(8):
                                P.mm(pvv[:, 0:n], wvl[:, k, fc * 128:(fc + 1) * 128], H2[:, k, t0 - b0:t0 - b0 + n],
                                     start=(k == 0), stop=(k == 7))
                            pgg = nps()
                            for k in range(8):
                                P.mm(pgg[:, 0:n], wgt[:, k, fc * 128:(fc + 1) * 128], H2[:, k, t0 - b0:t0 - b0 + n],
                                     start=(k == 0), stop=(k == 7))
                            gp, acc, sl = (GPC if t0 < TCX else GP[cnt % 2]), ACC[cnt % 2], SL[cnt % 2]
                            cnt += 1
                            P.copy("act", gp_view(gp, t0, n, 1), rows_view(pgg[:, 0:n], t0, n))
                            accv = rows_view(acc[:, 0:n], t0, n)
                            P.ts("dve", accv, gp_view(gp, t0, n, 0), ffnw[:, l, f, 0:1], ALU.mult)
                            P.stt(accv, gp_view(gp, t0, n, 1), ffnw[:, l, f, 1:2], accv, ALU.mult, ALU.add)
                            P.stt(accv, gp_view(gp, t0, n, 2), ffnw[:, l, f, 2:3], accv, ALU.mult, ALU.add)
                            P.act(sl[:, 0:n], acc[:, 0:n], AF.Silu, bias=ffnb[:, l, f:f + 1], scale=1.0)
                            P.tt("dve", G[:, f, t0 - b0:t0 - b0 + n], sl[:, 0:n], pvv[:, 0:n], ALU.mult)
                if stop == 'f2':
                    return
                HS.reset()
                SQT = [HS.alloc([512], BF16) for _ in range(2)]
                YF2 = [HS.alloc([512], F32) for _ in range(2)]
                RS3 = [HS.alloc([512], F32) for _ in range(3)]
                TM3 = [HS.alloc([512], F32) for _ in range(2)]
                stat = [ps[3], ps[4], ps[5]]
                cnt = 0
                for d in range(8):
                    (wd0,) = wload([w_down[l][0:1408, d * 128:(d + 1) * 128].rearrange("(k p) c -> p k c", p=128)], 11)
                    (wd1,) = wload([w_down[l][1408:2816, d * 128:(d + 1) * 128].rearrange("(k p) c -> p k c", p=128)], 11)
                    for gi, (t0, n) in enumerate(blk):
                        pd = nps((0, 1, 2))
                        for kf in range(NFC):
                            wsl = wd0[:, kf, :] if kf < 11 else wd1[:, kf - 11, :]
                            P.mm(pd[:, 0:n], wsl, G[:, kf, t0 - b0:t0 - b0 + n], start=(kf == 0), stop=(kf == NFC - 1))
                        P.act(YF2[cnt % 2][:, 0:n], pd[:, 0:n], AF.Copy, scale=1.0)
                        P.copy("dve", H2[:, d, t0 - b0:t0 - b0 + n], YF2[cnt % 2][:, 0:n])
                        P.tt("pool", SQT[cnt % 2][:, 0:n], YF2[cnt % 2][:, 0:n], YF2[cnt % 2][:, 0:n], ALU.mult)
                        P.mm(stat[gi][:, 0:n], onesb, SQT[cnt % 2][:, 0:n], start=(d == 0), stop=(d == 7))
                        cnt += 1
                if stop == 'f3':
                    return
                for gi, (t0, n) in enumerate(blk):
                    w = 1 if t0 < TCX else 0
                    rstd_from(stat[gi][:, 0:n], RS3[gi][:, 0:n], 1.0 / D)
                    for d in range(8):
                        P.tt("dve", TM3[d % 2][:, 0:n], H2[:, d, t0 - b0:t0 - b0 + n], RS3[gi][:, 0:n], ALU.mult)
                        P.stt(X[:, d, t0:t0 + n], TM3[d % 2][:, 0:n], der[:, l * 4 + 3, d, w:w + 1], X[:, d, t0:t0 + n],
                              ALU.mult, ALU.add)

        for l in range(depth):
            layer(l)
        for k in range(8):
            P.store(outT[k * 128:(k + 1) * 128, :], X[:, k, :], sem_o)
        P.finalize()
    return nc


def _host_layout(inputs):
    f = lambda a: np.ascontiguousarray(np.asarray(a, dtype=np.float32))
    x, c, ctx, c_ctx = f(inputs["x"]), f(inputs["c"]), f(inputs["ctx"]), f(inputs["c_ctx"])
    B = x.shape[0]
    vecs = np.zeros((128, NVEC), np.float32)
    ng = f(inputs["norm_g"]).reshape(DEPTH, 4, 8, 128)
    vecs[:, V_NORMG:V_NORMG + 128] = ng.transpose(3, 0, 1, 2).reshape(128, -1)
    ba = f(inputs["b_ada"]).reshape(DEPTH, 48, 128)
    vecs[:, V_BADA:V_BADA + 192] = ba.transpose(2, 0, 1).reshape(128, -1)
    cw = f(inputs["conv_w"]).reshape(DEPTH, 31, 4, 128)
    vecs[:, V_CONVW:V_CONVW + 496] = cw.transpose(3, 0, 2, 1).reshape(128, -1)
    for name, off in (("conv_b", V_CONVB), ("conv_ln_g", V_LNG), ("conv_ln_b", V_LNB)):
        vecs[:, off:off + 16] = f(inputs[name]).reshape(DEPTH, 4, 128).transpose(2, 0, 1).reshape(128, -1)
    fw = f(inputs["ffn_conv_w"]).reshape(DEPTH, 3, NFC, 128)
    vecs[:, V_FFNW:V_FFNW + 264] = fw.transpose(3, 0, 2, 1).reshape(128, -1)
    vecs[:, V_FFNB:V_FFNB + 88] = f(inputs["ffn_conv_b"]).reshape(DEPTH, NFC, 128).transpose(2, 0, 1).reshape(128, -1)
    vecs[:, V_IDENT:V_IDENT + 128] = np.eye(128, dtype=np.float32)
    vecs[:, V_ONES:V_ONES + 128] = 1.0
    s_idx = np.arange(128)[:, None]
    j_idx = np.arange(128)[None, :]
    vecs[:, V_TRIF:V_TRIF + 128] = (s_idx <= j_idx).astype(np.float32)
    vecs[:, V_TRIB:V_TRIB + 128] = (s_idx >= j_idx).astype(np.float32)
    lvecs = np.zeros((DEPTH, 128, NLV), np.float32)
    lvecs[:, :, 0:512] = f(inputs["mlstm_norm_g"])[:, None, :]
    bg = f(inputs["b_gates"]).reshape(DEPTH, 2, 2, 4)
    lvecs[:, :, 512:656] = np.broadcast_to(bg[:, None, None, :, 0, :], (DEPTH, 128, 18, 2, 4)).reshape(DEPTH, 128, 144)
    lvecs[:, :, 656:800] = np.broadcast_to(bg[:, None, None, :, 1, :], (DEPTH, 128, 18, 2, 4)).reshape(DEPTH, 128, 144)
    shared = {"vecs": vecs, "lvecs": lvecs, "w_ada": f(inputs["w_ada"]), "w_in": f(inputs["w_in"]),
              "wproj": f(inputs["w_out"]), "w_up": f(inputs["w_up"]), "w_down": f(inputs["w_down"])}
    maps = []
    for b in range(B):
        m = dict(shared)
        m["xT"] = np.ascontiguousarray(np.concatenate([ctx[b], x[b]], axis=0).T)
        cvv = np.stack([c[b].reshape(8, 128).T, c_ctx.reshape(8, 128).T], axis=-1)
        m["cvec"] = np.ascontiguousarray(cvv.reshape(128, 16))
        maps.append(m)
    return maps


_NC_CACHE = {}
_DBG = {}


def kernel(**inputs):
    maps = _host_layout(inputs)
    if "nc" not in _NC_CACHE:
        _NC_CACHE["nc"] = build(DEPTH)
    res = run_bass_kernel_spmd(_NC_CACHE["nc"], maps, core_ids=list(range(len(maps))))
    out = np.stack([np.ascontiguousarray(r["outT"][:, TCX:].T) for r in res.results], axis=0)
    return out.astype(np.float32)
```

```python
import bisect
from contextlib import ExitStack

import numpy as np
import concourse.bass as bass
import concourse.mybir as mybir
from concourse.bass_utils import run_bass_kernel_spmd

F32 = mybir.dt.float32
BF16 = mybir.dt.bfloat16
AF = mybir.ActivationFunctionType
ALU = mybir.AluOpType
AX = mybir.AxisListType

D = 1024
NT = 2304
TCX = 256
DEPTH = 4
DFF = 2816
NFC = 22
EPS = 1e-6
KSCALE = 128 ** -0.5
_ESZ = {F32: 4, BF16: 2}


def _esz(dt):
    return _ESZ[dt]


def ap_intervals(ap):
    if ap.tensor.name.startswith("ps"):
        return [(0, 2048)]
    esz = _esz(ap.dtype)
    dims = list(ap.ap)
    pstride = dims[0][0]
    off = ap.offset
    free_off = off % pstride if pstride > 0 else off
    fd = [(abs(s), c) for (s, c) in dims[1:] if c > 1 and s != 0]
    if not fd:
        return [(free_off * esz, (free_off + 1) * esz)]
    fd.sort()
    s0, c0 = fd[0]
    span = (c0 - 1) * s0 + 1
    runs = [free_off]
    for (s, c) in fd[1:]:
        if len(runs) == 1 and s <= span:
            span = (c - 1) * s + span
        else:
            if len(runs) * c > 64:
                lo = runs[0]
                hi = runs[-1] + span + (c - 1) * s
                runs = [lo]
                span = hi - lo
            else:
                runs = [r + i * s for i in range(c) for r in runs]
                runs.sort()
    return [(r * esz, (r + span) * esz) for r in runs]


class _IMap:
    def __init__(self):
        self.starts = [0]
        self.segs = [[None, {}]]

    def _split(self, pos):
        i = bisect.bisect_right(self.starts, pos) - 1
        if self.starts[i] == pos:
            return i
        w, r = self.segs[i]
        self.starts.insert(i + 1, pos)
        self.segs.insert(i + 1, [w, dict(r)])
        return i + 1

    def rng(self, lo, hi):
        i = self._split(lo)
        j = self._split(hi)
        return range(i, j)


class Prog:
    ENGS = ("pe", "act", "dve", "pool", "sp")

    def __init__(self, nc, es):
        self.nc = nc
        self.es = es
        self.sem = {e: es.enter_context(nc.semaphore("s_" + e)) for e in ("pe", "act", "dve", "pool")}
        self.tick = {e: 0 for e in self.ENGS}
        self.ops = {e: [] for e in self.ENGS}
        self.seen = {e: {} for e in self.ENGS}
        self.dsem = []
        self.maps = {}
        self.nops = 0

    def dma_slot(self):
        s = self.es.enter_context(self.nc.semaphore("d%d" % len(self.dsem)))
        self.dsem.append([s, 0])
        return len(self.dsem) - 1

    def _segs(self, ap):
        m = self.maps.get(ap.tensor.name)
        if m is None:
            m = self.maps[ap.tensor.name] = _IMap()
        for lo, hi in ap_intervals(ap):
            for i in m.rng(lo, hi):
                yield m.segs[i]

    def emit(self, eng, fn, reads, writes, sig=True, dma=None):
        deps = {}

        def add(sv):
            if sv is None:
                return
            s, v = sv
            if s[0] == "d":
                v = self.dsem[s[1]][1]
            if deps.get(s, 0) < v:
                deps[s] = v

        rsegs = [sg for ap in reads for sg in self._segs(ap)]
        wsegs = [sg for ap in writes for sg in self._segs(ap)]
        for sg in rsegs:
            add(sg[0])
        for sg in wsegs:
            add(sg[0])
            for s, v in sg[1].items():
                add((s, v))
        if dma is not None:
            self.dsem[dma][1] += 16
            me = (("d", dma), self.dsem[dma][1])
        elif sig:
            self.tick[eng] += 1
            me = (("e", eng), self.tick[eng])
        else:
            me = (("e", eng), self.tick[eng] + 1)
        waits = []
        for s, v in deps.items():
            if s == ("e", "pe") and eng == "pe":
                continue
            if self.seen[eng].get(s, 0) >= v:
                continue
            self.seen[eng][s] = v
            waits.append((s, v))
        for sg in rsegs:
            if sg[1].get(me[0], 0) < me[1]:
                sg[1][me[0]] = me[1]
        for sg in wsegs:
            sg[0] = me
            sg[1] = {}
        self.ops[eng].append((fn, waits, (dma if dma is not None else (eng if sig else None))))
        self.nops += 1

    def mm(self, out, lhsT, rhs, start=True, stop=True):
        assert int(np.prod(out.shape[1:])) == int(np.prod(rhs.shape[1:])), (out.shape, rhs.shape)
        assert out.shape[0] == int(np.prod(lhsT.shape[1:])), (out.shape, lhsT.shape)
        assert lhsT.shape[0] == rhs.shape[0], (lhsT.shape, rhs.shape)
        self.emit("pe", lambda e: e.matmul(out, lhsT, rhs, start=start, stop=stop), [lhsT, rhs], [out], sig=stop)

    def tr(self, out, in_, ident):
        self.emit("pe", lambda e: e.transpose(out, in_, ident), [in_, ident], [out], sig=True)

    def act(self, out, in_, func, bias=None, scale=None):
        kw = {}
        rd = [in_]
        if bias is not None:
            kw["bias"] = bias
            if not isinstance(bias, (int, float)):
                rd.append(bias)
        if scale is not None:
            kw["scale"] = scale
            if not isinstance(scale, (int, float)):
                rd.append(scale)
        self.emit("act", lambda e: e.activation(out, in_, func, **kw), rd, [out])

    def tt(self, eng, out, in0, in1, op):
        self.emit(eng, lambda e: e.tensor_tensor(out, in0, in1, op), [in0, in1], [out])

    def ts(self, eng, out, in0, s1, op0, s2=None, op1=None):
        rd = [in0]
        if not isinstance(s1, (int, float)):
            rd.append(s1)
        if s2 is not None and not isinstance(s2, (int, float)):
            rd.append(s2)
        if op1 is None:
            self.emit(eng, lambda e: e.tensor_scalar(out, in0, s1, None, op0), rd, [out])
        else:
            self.emit(eng, lambda e: e.tensor_scalar(out, in0, s1, s2, op0, op1), rd, [out])

    def stt(self, out, in0, sc, in1, op0, op1):
        rd = [in0, in1]
        if not isinstance(sc, (int, float)):
            rd.append(sc)
        self.emit("dve", lambda e: e.scalar_tensor_tensor(out, in0, sc, in1, op0, op1), rd, [out])

    def copy(self, eng, out, in_):
        if eng == "act":
            self.emit("act", lambda e: e.copy(out, in_), [in_], [out])
        else:
            self.emit(eng, lambda e: e.tensor_copy(out, in_), [in_], [out])

    def memset(self, eng, out, val):
        self.emit(eng, lambda e: e.memset(out, val), [], [out])

    def recip(self, out, in_):
        self.emit("dve", lambda e: e.reciprocal(out, in_), [in_], [out])

    def load(self, out, in_, slot, q="sp"):
        self.emit(q, lambda e: e.dma_start(out=out, in_=in_), [], [out], dma=slot)

    def store(self, out, in_, slot, q="sp"):
        self.emit(q, lambda e: e.dma_start(out=out, in_=in_), [in_], [], dma=slot)

    def finalize(self):
        nc = self.nc
        fin = []
        for e in ("pe", "act", "dve", "pool"):
            if self.tick[e] > 0:
                fin.append((("e", e), self.tick[e]))
        for i, (s, c) in enumerate(self.dsem):
            if c > 0:
                fin.append((("d", i), c))
        self.ops["sp"].append((None, fin, None))

        def semof(s):
            return self.sem[s[1]] if s[0] == "e" else self.dsem[s[1]][0]

        def run(e, lst):
            for fn, waits, inc in lst:
                for s, v in waits:
                    e.wait_ge(semof(s), v)
                if fn is None:
                    continue
                ins = fn(e)
                if inc is None:
                    continue
                if isinstance(inc, int):
                    ins.then_inc(self.dsem[inc][0], 16)
                else:
                    ins.then_inc(self.sem[inc], 1)

        with nc.Block() as block:
            @block.tensor
            def _(e):
                run(e, self.ops["pe"])

            @block.scalar
            def _(e):
                run(e, self.ops["act"])

            @block.vector
            def _(e):
                run(e, self.ops["dve"])

            @block.gpsimd
            def _(e):
                run(e, self.ops["pool"])

            @block.sync
            def _(e):
                run(e, self.ops["sp"])


class Arena:
    def __init__(self, nc, es, name, nbytes):
        self.t = es.enter_context(nc.sbuf_tensor(name, [128, nbytes // 4], F32))
        self.ap = self.t[:, :] if not hasattr(self.t, "ap") else self.t.ap()
        self.cap = nbytes
        self.top = 0

    def at(self, off, shape, dt):
        n = int(np.prod(shape))
        nb = n * _esz(dt)
        assert off % 4 == 0 and off + nb <= self.cap, (off, nb, self.cap)
        a = self.ap[:, off // 4:(off + nb + 3) // 4]
        if dt != F32:
            a = a.bitcast(dt)[:, 0:n]
        if len(shape) == 2:
            a = a.rearrange("p (a b) -> p a b", b=shape[1])
        elif len(shape) == 3:
            a = a.rearrange("p (a b c) -> p a b c", b=shape[1], c=shape[2])
        elif len(shape) == 4:
            a = a.rearrange("p (a b c d) -> p a b c d", b=shape[1], c=shape[2], d=shape[3])
        return a

    def alloc(self, shape, dt):
        off = (self.top + 31) // 32 * 32
        n = int(np.prod(shape)) * _esz(dt)
        self.top = off + (n + 3) // 4 * 4
        assert self.top <= self.cap, ("arena overflow", self.top, self.cap)
        return self.at(off, shape, dt)


class Sub:
    def __init__(self, A, base, size):
        self.A, self.base, self.size, self.top = A, base, size, 0

    def reset(self):
        self.top = 0

    def alloc(self, shape, dt):
        off = (self.top + 31) // 32 * 32
        n = int(np.prod(shape)) * _esz(dt)
        self.top = off + (n + 3) // 4 * 4
        assert self.top <= self.size, ("sub overflow", self.top, self.size)
        return self.A.at(self.base + off, shape, dt)


V_NORMG, V_BADA, V_CONVW, V_CONVB, V_LNG, V_LNB, V_FFNW, V_FFNB = 0, 128, 320, 816, 832, 848, 864, 1128
V_IDENT, V_ONES, V_TRIF, V_TRIB, NVEC = 1216, 1344, 1472, 1600, 1728
NLV = 800
SUBG = [(i * 256, 256) for i in range(9)]
GROUPS = [(0, 256)] + [(256 + 512 * i, 512) for i in range(4)]
FBLOCKS = [[(0, 256), (256, 512), (768, 384)], [(1152, 512), (1664, 512), (2176, 128)]]
ORDER = [list(range(18)), [1, 0] + list(range(17, 1, -1))]


_DBG = {}


def build(depth=DEPTH, stop=None):
    nc = bass.Bass("TRN2", target_bir_lowering=False)
    xT = nc.dram_tensor("xT", [D, NT], F32, kind="ExternalInput").ap()
    cvec_d = nc.dram_tensor("cvec", [128, 16], F32, kind="ExternalInput").ap()
    vecs_d = nc.dram_tensor("vecs", [128, NVEC], F32, kind="ExternalInput").ap()
    lvecs_d = nc.dram_tensor("lvecs", [DEPTH, 128, NLV], F32, kind="ExternalInput").ap()
    w_ada = nc.dram_tensor("w_ada", [DEPTH, D, 6 * D], F32, kind="ExternalInput").ap()
    w_in = nc.dram_tensor("w_in", [DEPTH, D, 3088], F32, kind="ExternalInput").ap()
    w_out = nc.dram_tensor("wproj", [DEPTH, D, D], F32, kind="ExternalInput").ap()
    w_up = nc.dram_tensor("w_up", [DEPTH, D, 2 * DFF], F32, kind="ExternalInput").ap()
    w_down = nc.dram_tensor("w_down", [DEPTH, DFF, D], F32, kind="ExternalInput").ap()
    outT = nc.dram_tensor("outT", [D, NT], F32, kind="ExternalOutput").ap()

    with ExitStack() as es:
        P = Prog(nc, es)
        A = Arena(nc, es, "arena", 211968)
        psl = [es.enter_context(nc.psum_tensor("ps%d" % i, [128, 512], F32)) for i in range(6)]
        ps = [p[:, :] for p in psl]
        psbs = [es.enter_context(nc.psum_tensor("psb%d" % i, [128, 1024], BF16))[:, :] for i in range(2)]
        pctr = [0]

        rot = [0, 1, 2, 3, 4, 5]

        def nps(lst=None):
            lst = rot if lst is None else lst
            pctr[0] += 1
            return ps[lst[pctr[0] % len(lst)]]

        bctr = [0]

        def npsb():
            bctr[0] += 1
            i = bctr[0] % 2
            return psbs[i][:, 0:128]

        X = A.alloc([8, NT], F32)
        V = A.alloc([NVEC], F32)
        LV = A.alloc([NLV], F32)
        CB = A.alloc([4, 128], BF16)
        identb, onesb, trifb, tribb = CB[:, 0, :], CB[:, 1, :], CB[:, 2, :], CB[:, 3, :]
        maskb = [trifb, tribb]
        identf, onesf = V[:, V_IDENT:V_IDENT + 128], V[:, V_ONES:V_ONES + 128]
        triff, tribf = V[:, V_TRIF:V_TRIF + 128], V[:, V_TRIB:V_TRIB + 128]
        cst = A.alloc([4], F32)
        c_eps, c_one = cst[:, 0:1], cst[:, 1:2]
        cv = A.alloc([8, 2], F32)
        sv = A.alloc([8, 2], F32)
        modv = A.alloc([DEPTH, 48, 2], F32)
        der = A.alloc([DEPTH * 4, 8, 2], F32)
        normg = V[:, V_NORMG:V_NORMG + 128].rearrange("p (l n k) -> p l n k", n=4, k=8)
        bada = V[:, V_BADA:V_BADA + 192].rearrange("p (l f) -> p l f", f=48)
        convw = V[:, V_CONVW:V_CONVW + 496].rearrange("p (l c k) -> p l c k", c=4, k=31)
        convb = V[:, V_CONVB:V_CONVB + 16].rearrange("p (l c) -> p l c", c=4)
        lng = V[:, V_LNG:V_LNG + 16].rearrange("p (l c) -> p l c", c=4)
        lnb = V[:, V_LNB:V_LNB + 16].rearrange("p (l c) -> p l c", c=4)
        ffnw = V[:, V_FFNW:V_FFNW + 264].rearrange("p (l f k) -> p l f k", f=22, k=3)
        ffnb = V[:, V_FFNB:V_FFNB + 88].rearrange("p (l f) -> p l f", f=22)
        mng = LV[:, 0:512]
        bgi = LV[:, 512:656].rearrange("p (t d h) -> p t d h", d=2, h=4)
        bgf = LV[:, 656:800].rearrange("p (t d h) -> p t d h", d=2, h=4)
        WST = [A.alloc([2048], F32) for _ in range(2)]
        WBF = [A.alloc([2048], BF16) for _ in range(4)]
        wst_sem = [P.dma_slot() for _ in range(2)]
        wctr = [0, 0]
        H_off = (A.top + 31) // 32 * 32
        H = A.alloc([8, NT], BF16)
        QM_off = (A.top + 31) // 32 * 32
        QM = A.alloc([4, NT], BF16)
        S_off = (A.top + 31) // 32 * 32
        S = Sub(A, S_off, A.cap - S_off)
        HS = Sub(A, H_off + 8 * 1152 * 2, 8 * 1152 * 2)
        G = A.at(QM_off, [NFC, 1152], BF16)
        H2 = A.at(H_off, [8, 1152], BF16)
        sem_x, sem_v, sem_lv, sem_o = P.dma_slot(), P.dma_slot(), P.dma_slot(), P.dma_slot()
        _DBG.update(H_off=H_off, QM_off=QM_off, S_off=S_off)

        def wload(pieces, nk):
            si = wctr[0] % 2
            wctr[0] += 1
            bi = wctr[1] % 4
            wctr[1] += 1
            off = 0
            outs = []
            for pc in pieces:
                ncol = pc.shape[2]
                n = nk * ncol
                stv = WST[si][:, off:off + n].rearrange("p (k c) -> p k c", c=ncol)
                P.load(stv, pc, wst_sem[si])
                outs.append(WBF[bi][:, off:off + n].rearrange("p (k c) -> p k c", c=ncol))
                off += n
            assert off <= 2048
            P.copy("pool", WBF[bi][:, 0:off], WST[si][:, 0:off])
            return outs

        def wcols(w3, l, c0, ncol):
            return w3[l].rearrange("(k p) c -> p k c", p=128)[:, :, c0:c0 + ncol]

        for k in range(8):
            P.load(X[:, k, :], xT[k * 128:(k + 1) * 128, :], sem_x)
        P.load(V, vecs_d, sem_v)
        P.load(cv, cvec_d.rearrange("p (k w) -> p k w", w=2), sem_v)
        P.memset("dve", cst[:, 0:1], EPS)
        P.memset("dve", cst[:, 1:2], 1.0)
        P.copy("dve", CB[:, 0, :], identf)
        P.copy("dve", CB[:, 1, :], onesf)
        P.copy("dve", CB[:, 2, :], triff)
        P.copy("dve", CB[:, 3, :], tribf)
        P.act(sv, cv, AF.Silu)
        def adaln_gen(l, pm):
            for j in range(24):
                si = wctr[0] % 2
                wctr[0] += 1
                stv = WST[si].rearrange("p (k c) -> p k c", c=256)
                P.load(stv, wcols(w_ada, l, j * 256, 256), wst_sem[si])
                for fc in range(2):
                    f = j * 2 + fc
                    for k in range(8):
                        P.mm(pm[:, 2 * f:2 * f + 2], stv[:, k, fc * 128:(fc + 1) * 128], sv[:, k, :],
                             start=(k == 0), stop=(k == 7))
                yield j

        def adaln_finish(l, pm):
            pm3 = pm[:, 0:96].rearrange("p (f w) -> p f w", w=2)
            for w in range(2):
                P.tt("dve", modv[:, l, :, w], pm3[:, :, w], bada[:, l, :], ALU.add)
            for w in range(2):
                P.stt(der[:, l * 4 + 0, :, w], modv[:, l, 8:16, w], 1.0, normg[:, l, 0, :], ALU.add, ALU.mult)
                P.tt("dve", der[:, l * 4 + 1, :, w], modv[:, l, 16:24, w], normg[:, l, 1, :], ALU.mult)
                P.stt(der[:, l * 4 + 2, :, w], modv[:, l, 32:40, w], 1.0, normg[:, l, 2, :], ALU.add, ALU.mult)
                P.tt("dve", der[:, l * 4 + 3, :, w], modv[:, l, 40:48, w], normg[:, l, 3, :], ALU.mult)

        for _ in adaln_gen(0, ps[5]):
            pass
        adaln_finish(0, ps[5])

        def split3(src, spl, spr, n, rows_=128):
            h = [spl[0:rows_, j, 0:n] for j in range(3)]
            r = spr[0:rows_, 0:n]
            P.copy("dve", h[0], src)
            P.tt("dve", r, src, h[0], ALU.subtract)
            P.copy("dve", h[1], r)
            P.tt("dve", r, r, h[1], ALU.subtract)
            P.copy("dve", h[2], r)
            return h

        def rstd_from(psum_ap, out_ap, inv_n):
            P.act(out_ap, psum_ap, AF.Ln, bias=c_eps, scale=inv_n)
            P.act(out_ap, out_ap, AF.Exp, scale=-0.5)

        def normmod(l, subs, kind_gs, sh_base, dst, sub):
            sq = [sub.alloc([8, 256], BF16) for _ in range(2)]
            rsb = [sub.alloc([256], F32) for _ in range(2)]
            tmp = [sub.alloc([256], F32) for _ in range(3)]
            for i, (t0, n) in enumerate(subs):
                w = 1 if t0 < TCX else 0
                pp = nps()
                for k in range(8):
                    P.tt("pool", sq[i % 2][:, k, 0:n], X[:, k, t0:t0 + n], X[:, k, t0:t0 + n], ALU.mult)
                for k in range(8):
                    P.mm(pp[:, 0:n], onesb, sq[i % 2][:, k, 0:n], start=(k == 0), stop=(k == 7))
                rstd_from(pp[:, 0:n], rsb[i % 2][:, 0:n], 1.0 / D)
                for k in range(8):
                    tt_ = tmp[k % 3][:, 0:n]
                    P.tt("dve", tt_, X[:, k, t0:t0 + n], rsb[i % 2][:, 0:n], ALU.mult)
                    P.act(dst(k, t0, n), tt_, AF.Identity, bias=modv[:, l, sh_base + k, w:w + 1],
                          scale=der[:, l * 4 + kind_gs, k, w:w + 1])

        def layer(l):
            P.load(LV, lvecs_d[l], sem_lv)
            if stop == 'setup':
                return
            S.reset()
            normmod(l, SUBG, 0, 0, lambda k, t0, n: H[:, k, t0:t0 + n], S)
            if stop == 'norm1':
                return
            S.reset()
            Kb = S.alloc([NT], BF16)
            VTM = S.alloc([18, 130], BF16)
            hs_off = S.base + (S.top + 31) // 32 * 32
            HSUM = S.alloc([18, 128], F32)
            GI = S.alloc([18, 2, 4], F32)
            LF = S.alloc([18, 2, 4], F32)
            NB = S.alloc([18, 2, 4], F32)
            DD = S.alloc([18, 2, 4], F32)
            rows = S.alloc([4, 144], F32)
            MB = S.alloc([288], F32)
            T3 = S.alloc([3, 144], F32)
            SPL = A.at(hs_off, [3, 288], BF16)
            SPR = A.at(hs_off + 2048, [288], F32)
            E3 = S.alloc([3, 144], F32)
            UG = E3[:, 0, :].rearrange("p (t d h) -> p t d h", d=2, h=4)
            WB = E3[:, 1, :].rearrange("p (t d h) -> p t d h", d=2, h=4)
            CL = E3[:, 2, :].rearrange("p (t d h) -> p t d h", d=2, h=4)
            Cst = [S.alloc([129], F32) for _ in range(2)]
            Csb = [[S.alloc([130], BF16) for _ in range(2)] for _ in range(2)]
            PT = [[S.alloc([128], BF16) for _ in range(2)] for _ in range(2)]
            KU = [[S.alloc([128], BF16) for _ in range(2)] for _ in range(2)]
            dn = S.alloc([8], F32)
            hdt = [S.alloc([128], F32) for _ in range(2)]
            stt_ = S.alloc([18, 6], F32)
            mv = S.alloc([18, 2], F32)
            rsd = S.alloc([18], F32)
            OG = [S.alloc([128], F32) for _ in range(2)]
            XN = hdt
            MT = [S.alloc([128], BF16) for _ in range(2)]

            (wg,) = wload([wcols(w_in, l, 3072, 16)], 8)
            pg = nps()
            for t in range(18):
                for k in range(8):
                    P.mm(pg[:, t * 16:(t + 1) * 16], H[:, k, t * 128:(t + 1) * 128], wg[:, k, :],
                         start=(k == 0), stop=(k == 7))
            pg5 = pg[:, 0:288].rearrange("p (t d g h) -> p t d g h", d=2, g=2, h=4)
            P.tt("dve", GI, pg5[:, :, :, 0, :], bgi, ALU.add)
            P.tt("dve", LF, pg5[:, :, :, 1, :], bgf, ALU.add)
            if stop == 'g1':
                return
            LFf = LF.rearrange("p t d h -> p (t d h)")
            GIf = GI.rearrange("p t d h -> p (t d h)")
            NBf = NB.rearrange("p t d h -> p (t d h)")
            DDf = DD.rearrange("p t d h -> p (t d h)")
            P.ts("dve", LFf, LFf, -60.0, ALU.max)
            P.act(LFf, LFf, AF.Exp, scale=-1.0)
            P.act(LFf, LFf, AF.Ln, bias=c_one, scale=1.0)
            if stop == 'g2a':
                return
            pc_ = nps()
            L3 = split3(LFf, SPL, SPR, 144)
            for j3, hb in enumerate(L3):
                P.mm(pc_[:, 0:144], trifb, hb, start=(j3 == 0), stop=(j3 == 2))
            pc2 = nps()
            for j3, hb in enumerate(L3):
                P.mm(pc2[:, 0:144], tribb, hb, start=(j3 == 0), stop=(j3 == 2))
            pc3 = nps()
            for j3, hb in enumerate(L3):
                P.mm(pc3[:, 0:144], onesb, hb, start=(j3 == 0), stop=(j3 == 2))
            pcf = pc_[:, 0:144].rearrange("p (t d h) -> p t d h", d=2, h=4)
            pcb = pc2[:, 0:144].rearrange("p (t d h) -> p t d h", d=2, h=4)
            P.copy("act", NB[:, :, 0, :], pcf[:, :, 0, :])
            P.copy("act", NB[:, :, 1, :], pcb[:, :, 1, :])
            P.tt("dve", DDf, GIf, NBf, ALU.add)
            if stop == 'g2':
                return
            P.emit("pool", lambda e: e.tensor_reduce(rows[0:1, 0, :], DDf, AX.C, ALU.max), [DDf], [rows[0:1, 0, :]])
            P.copy("act", rows[0:1, 1, :], pc3[0:1, 0:144])
            P.memset("dve", rows[0:1, 3, :], 0.0)
            if stop == 'g3':
                return
            r4 = rows[0:1, :, :].rearrange("p r (t d h) -> p r t d h", d=2, h=4)
            for dr in range(2):
                od = ORDER[dr]
                for i, t in enumerate(od):
                    P.tt("dve", r4[:, 2, t, dr, :], r4[:, 3, t, dr, :], r4[:, 0, t, dr, :], ALU.max)
                    if i + 1 < 18:
                        P.tt("dve", r4[:, 3, od[i + 1], dr, :], r4[:, 2, t, dr, :], r4[:, 1, t, dr, :], ALU.subtract)
            if stop == 'g4':
                return
            pmb = nps()
            R3 = split3(rows[0:1, 2:4, :].rearrange("p r n -> p (r n)"), SPL, SPR, 288, rows_=1)
            for j3, hb in enumerate(R3):
                P.mm(pmb[:, 0:288], onesb[0:1, :], hb, start=(j3 == 0), stop=(j3 == 2))
            P.copy("act", MB, pmb[:, 0:288])
            P.tt("dve", T3[:, 0, :], DDf, MB[:, 0:144], ALU.subtract)
            P.tt("dve", T3[:, 1, :], MB[:, 144:288], MB[:, 0:144], ALU.subtract)
            P.tt("dve", T3[:, 2, :], NBf, MB[:, 0:144], ALU.subtract)
            P.ts("dve", T3[:, 2, :], T3[:, 2, :], 80.0, ALU.min)
            P.act(E3.rearrange("p a b -> p (a b)"), T3.rearrange("p a b -> p (a b)"), AF.Exp)
            P.memset("pool", VTM[:, :, 128:130], 1.0)

            if stop == 'gates':
                return
            ag = None
            if l + 1 < depth:
                rot[:] = [0, 1, 2, 3, 4]
                ag = adaln_gen(l + 1, ps[5])
            for hd in range(4):
                wq, wk = wload([wcols(w_in, l, 1024 + hd * 128, 128), wcols(w_in, l, 1536 + hd * 128, 128)], 8)
                wv, wo = wload([wcols(w_in, l, 2048 + hd * 128, 128), wcols(w_in, l, 2560 + hd * 128, 128)], 8)
                for (t0, n) in GROUPS:
                    pq = nps()
                    for k in range(8):
                        P.mm(pq[:, 0:n], wq[:, k, :], H[:, k, t0:t0 + n], start=(k == 0), stop=(k == 7))
                    P.act(QM[:, hd, t0:t0 + n], pq[:, 0:n], AF.Copy, scale=KSCALE)
                    pk = nps()
                    for k in range(8):
                        P.mm(pk[:, 0:n], wk[:, k, :], H[:, k, t0:t0 + n], start=(k == 0), stop=(k == 7))
                    P.copy("dve", Kb[:, t0:t0 + n], pk[:, 0:n])
                for t4 in range(0, 18, 4):
                    nt_ = min(4, 18 - t4)
                    pv = nps()
                    for i in range(nt_):
                        t = t4 + i
                        for k in range(8):
                            P.mm(pv[:, i * 128:(i + 1) * 128], H[:, k, t * 128:(t + 1) * 128], wv[:, k, :],
                                 start=(k == 0), stop=(k == 7))
                    P.copy("act", VTM[:, t4:t4 + nt_, 0:128],
                           pv[:, 0:nt_ * 128].rearrange("p (a b) -> p a b", b=128))
                P.memset("pool", HSUM, 0.0)
                for dr in range(2):
                    P.memset("pool", Cst[dr], 0.0)
                for step in range(18):
                    for dr in range(2):
                        t = ORDER[dr][step]
                        par = step % 2
                        tk = slice(t * 128, (t + 1) * 128)
                        u_ap = UG[:, t, dr, hd:hd + 1]
                        w_ap = WB[:, t, dr, hd:hd + 1]
                        pS = nps()
                        P.mm(pS[:, 0:128], Kb[:, tk], QM[:, hd, tk])
                        pT = npsb()
                        P.tr(pT, Kb[:, tk], identb)
                        P.stt(PT[dr][par], pS[:, 0:128], u_ap, maskb[dr], ALU.mult, ALU.mult)
                        P.act(KU[dr][par], pT, AF.Copy, scale=u_ap)
                        P.ts("pool", Csb[dr][par][:, 0:129], Cst[dr], w_ap, ALU.mult)
                        pN = nps()
                        P.mm(pN[:, 0:129], PT[dr][par], VTM[:, t, 0:129], start=True, stop=False)
                        P.mm(pN[:, 0:129], QM[:, hd, tk], Csb[dr][par][:, 0:129], start=False, stop=True)
                        pC = nps()
                        P.mm(pC[:, 0:129], KU[dr][par], VTM[:, t, 0:129])
                        P.stt(Cst[dr], Cst[dr], w_ap, pC[:, 0:129], ALU.mult, ALU.add)
                        dcol = dn[:, dr * 2:dr * 2 + 1]
                        rcol = dn[:, dr * 2 + 1:dr * 2 + 2]
                        P.ts("dve", dcol, pN[:, 128:129], -1.0, ALU.mult, CL[:, t, dr, hd:hd + 1], ALU.max)
                        P.ts("dve", dcol, pN[:, 128:129], dcol, ALU.max)
                        P.recip(rcol, dcol)
                        P.act(hdt[dr], pN[:, 0:128], AF.Copy, scale=rcol)
                        P.tt("pool", HSUM[:, t, :], HSUM[:, t, :], hdt[dr], ALU.add)
                    if ag is not None and step % 3 == 2:
                        next(ag, None)
                for t in range(18):
                    P.emit("dve", lambda e, t=t: e.bn_stats(stt_[:, t, :], HSUM[:, t, :]), [HSUM[:, t, :]], [stt_[:, t, :]])
                    P.emit("dve", lambda e, t=t: e.bn_aggr(mv[:, t, :], stt_[:, t, :]), [stt_[:, t, :]], [mv[:, t, :]])
                P.act(rsd, mv[:, :, 1], AF.Ln, bias=c_eps, scale=1.0)
                P.act(rsd, rsd, AF.Exp, scale=-0.5)
                for t4 in range(0, 18, 4):
                    nt_ = min(4, 18 - t4)
                    pbs = []
                    for i in range(nt_):
                        t = t4 + i
                        po = nps()
                        for k in range(8):
                            P.mm(po[:, 0:128], H[:, k, t * 128:(t + 1) * 128], wo[:, k, :], start=(k == 0), stop=(k == 7))
                        P.act(OG[t % 2], po[:, 0:128], AF.Sigmoid)
                        P.tt("pool", OG[t % 2], OG[t % 2], mng[:, hd * 128:(hd + 1) * 128], ALU.mult)
                        P.ts("dve", XN[t % 2], HSUM[:, t, :], mv[:, t, 0:1], ALU.subtract, rsd[:, t:t + 1], ALU.mult)
                        P.tt("dve", MT[t % 2], XN[t % 2], OG[t % 2], ALU.mult)
                        pb = npsb()
                        P.tr(pb, MT[t % 2], identb)
                        P.copy("act", QM[:, hd, t * 128:(t + 1) * 128], pb)

            if ag is not None:
                for _ in ag:
                    pass
                adaln_finish(l + 1, ps[5])
                rot[:] = [0, 1, 2, 3, 4, 5]
            if stop == 'mlstm':
                return
            S.reset()
            UB = S.alloc([4, NT], BF16)
            s_mark = S.top
            UP = S.alloc([2880], BF16)
            SG = [S.alloc([512], F32) for _ in range(2)]
            P.memset("pool", UP, 0.0)

            def up_view(t0, n, shift):
                if t0 < TCX:
                    return UP[:, 16 + shift:16 + shift + 256]
                r0 = (t0 - TCX) // 64
                base = 288 + 80 * r0 + shift
                rws = n // 64
                return UP[:, base:base + 80 * rws].rearrange("p (r c) -> p r c", c=80)[:, :, 0:64]

            def rows_view(ap2, t0, n):
                if t0 < TCX:
                    return ap2
                return ap2.rearrange("p (r c) -> p r c", c=64)

            for c in range(4):
                wa, wgl = wload([wcols(w_in, l, c * 128, 128), wcols(w_in, l, 512 + c * 128, 128)], 8)
                if c == 0:
                    DBt = S.alloc([31, 128], BF16)
                for kk in range(31):
                    P.ts("pool" if kk % 2 else "dve", DBt[:, kk, :], identb, convw[:, l, c, kk:kk + 1], ALU.mult)
                for gi, (t0, n) in enumerate(GROUPS):
                    pa = nps()
                    for k in range(8):
                        P.mm(pa[:, 0:n], wa[:, k, :], H[:, k, t0:t0 + n], start=(k == 0), stop=(k == 7))
                    pgl = nps()
                    for k in range(8):
                        P.mm(pgl[:, 0:n], wgl[:, k, :], H[:, k, t0:t0 + n], start=(k == 0), stop=(k == 7))
                    P.act(SG[gi % 2][:, 0:n], pgl[:, 0:n], AF.Sigmoid)
                    P.tt("dve", up_view(t0, n, 0), rows_view(pa[:, 0:n], t0, n), rows_view(SG[gi % 2][:, 0:n], t0, n), ALU.mult)
                    py = nps()
                    for kk in range(31):
                        P.mm(rows_view(py[:, 0:n], t0, n), DBt[:, kk, :], up_view(t0, n, kk - 15),
                             start=(kk == 0), stop=(kk == 30))
                    P.act(UB[:, c, t0:t0 + n], py[:, 0:n], AF.Identity, bias=convb[:, l, c:c + 1], scale=1.0)
            if stop == 'c1':
                return
            S.top = s_mark
            SQ = [S.alloc([4, 256], BF16) for _ in range(2)]
            MU = [S.alloc([256], F32) for _ in range(2)]
            RS = [S.alloc([256], F32) for _ in range(2)]
            TM = [S.alloc([256], F32) for _ in range(2)]
            for i, (t0, n) in enumerate(SUBG):
                p1, p2 = nps(), nps()
                for c in range(4):
                    P.act(SQ[i % 2][:, c, :], UB[:, c, t0:t0 + n], AF.Square)
                for c in range(4):
                    P.mm(p1[:, 0:n], onesb, UB[:, c, t0:t0 + n], start=(c == 0), stop=(c == 3))
                for c in range(4):
                    P.mm(p2[:, 0:n], onesb, SQ[i % 2][:, c, :], start=(c == 0), stop=(c == 3))
                mu, rs = MU[i % 2], RS[i % 2]
                P.act(mu, p1[:, 0:n], AF.Copy, scale=1.0 / 512)
                P.tt("dve", rs, mu, mu, ALU.mult)
                P.stt(rs, p2[:, 0:n], 1.0 / 512, rs, ALU.mult, ALU.subtract)
                P.ts("dve", rs, rs, 0.0, ALU.max)
                P.act(rs, rs, AF.Ln, bias=c_eps, scale=1.0)
                P.act(rs, rs, AF.Exp, scale=-0.5)
                for c in range(4):
                    tm = TM[c % 2]
                    P.tt("dve", tm, UB[:, c, t0:t0 + n], mu, ALU.subtract)
                    P.tt("pool", tm, tm, rs, ALU.mult)
                    P.act(UB[:, c, t0:t0 + n], tm, AF.Silu, bias=lnb[:, l, c:c + 1], scale=lng[:, l, c:c + 1])

            if stop == 'conv':
                return
            wo4 = []
            for i in range(4):
                (wv_,) = wload([wcols(w_out, l, i * 256, 256)], 8)
                wo4.append(wv_)
            S.top = s_mark
            YS = [S.alloc([8, 256], BF16) for _ in range(1)]
            YF = S.alloc([8, 256], F32)
            RS2 = [S.alloc([256], F32) for _ in range(2)]
            TM2 = [S.alloc([256], F32) for _ in range(2)]
            for i, (t0, n) in enumerate(SUBG):
                w = 1 if t0 < TCX else 0
                for dh in range(2):
                    for d in range(4 * dh, 4 * dh + 4):
                        yo = ps[d % 4][:, 0:n]
                        for k in range(8):
                            rhs = UB[:, k, t0:t0 + n] if k < 4 else QM[:, k - 4, t0:t0 + n]
                            P.mm(yo, wo4[d // 2][:, k, (d % 2) * 128:(d % 2) * 128 + 128], rhs, start=(k == 0), stop=(k == 7))
                    for d in range(4 * dh, 4 * dh + 4):
                        yo = ps[d % 4][:, 0:n]
                        P.act(YF[:, d, :], yo, AF.Copy, scale=1.0)
                        P.tt("pool", YS[0][:, d, :], YF[:, d, :], YF[:, d, :], ALU.mult)
                pst = ps[4 + i % 2]
                for d in range(8):
                    P.mm(pst[:, 0:n], onesb, YS[0][:, d, :], start=(d == 0), stop=(d == 7))
                if stop == 'w2':
                    continue
                rstd_from(pst[:, 0:n], RS2[i % 2], 1.0 / D)
                for d in range(8):
                    P.tt("dve", TM2[d % 2], YF[:, d, :], RS2[i % 2], ALU.mult)
                    if stop != 'w3':
                        P.stt(X[:, d, t0:t0 + n], TM2[d % 2], der[:, l * 4 + 1, d, w:w + 1], X[:, d, t0:t0 + n], ALU.mult, ALU.add)

            if stop == 'wout':
                return
            for blk in FBLOCKS:
                b0 = blk[0][0]
                HS.reset()
                subs = [(b0 + i * 256, 256) for i in range(1152 // 256)] + [(b0 + 1024, 128)]
                normmod(l, subs, 2, 24, lambda k, t0, n: H2[:, k, t0 - b0:t0 - b0 + n], HS)
                if stop == 'f1':
                    return
                HS.reset()
                GP = [HS.alloc([528], F32) for _ in range(2)]
                ACC = [HS.alloc([512], F32) for _ in range(2)]
                SL = [HS.alloc([512], F32) for _ in range(2)]
                GPC = HS.alloc([264], F32)
                for gp in GP + [GPC]:
                    P.memset("pool", gp, 0.0)

                def gp_view(gp, t0, n, sh):
                    if t0 < TCX:
                        return gp[:, sh:sh + 256]
                    rws = n // 64
                    return gp[:, 0:66 * rws].rearrange("p (r c) -> p r c", c=66)[:, :, sh:sh + 64]

                cnt = 0
                for fp in range(11):
                    (wvl,) = wload([wcols(w_up, l, fp * 256, 256)], 8)
                    (wgt,) = wload([wcols(w_up, l, DFF + fp * 256, 256)], 8)
                    for fc in range(2):
                        f = fp * 2 + fc
                        for (t0, n) in blk:
                            pvv = nps()
                            for k in range(8):
                                P.mm(pvv[:, 0:n], wvl[:, k, fc * 128:(fc + 1) * 128], H2[:, k, t0 - b0:t0 - b0 + n],
                                     start=(k == 0), stop=(k == 7))
                            pgg = nps()
                            for k in range(8):
                                P.mm(pgg[:, 0:n], wgt[:, k, fc * 128:(fc + 1) * 128], H2[:, k, t0 - b0:t0 - b0 + n],
                                     start=(k == 0), stop=(k == 7))
                            gp, acc, sl = (GPC if t0 < TCX else GP[cnt % 2]), ACC[cnt % 2], SL[cnt % 2]
                            cnt += 1
                            P.copy("act", gp_view(gp, t0, n, 1), rows_view(pgg[:, 0:n], t0, n))
                            accv = rows_view(acc[:, 0:n], t0, n)
                            P.ts("dve", accv, gp_view(gp, t0, n, 0), ffnw[:, l, f, 0:1], ALU.mult)
                            P.stt(accv, gp_view(gp, t0, n, 1), ffnw[:, l, f, 1:2], accv, ALU.mult, ALU.add)
                            P.stt(accv, gp_view(gp, t0, n, 2), ffnw[:, l, f, 2:3], accv, ALU.mult, ALU.add)
                            P.act(sl[:, 0:n], acc[:, 0:n], AF.Silu, bias=ffnb[:, l, f:f + 1], scale=1.0)
                            P.tt("dve", G[:, f, t0 - b0:t0 - b0 + n], sl[:, 0:n], pvv[:, 0:n], ALU.mult)
                if stop == 'f2':
                    return
                HS.reset()
                SQT = [HS.alloc([512], BF16) for _ in range(2)]
                YF2 = [HS.alloc([512], F32) for _ in range(2)]
                RS3 = [HS.alloc([512], F32) for _ in range(3)]
                TM3 = [HS.alloc([512], F32) for _ in range(2)]
                stat = [ps[3], ps[4], ps[5]]
                cnt = 0
                for d in range(8):
                    (wd0,) = wload([w_down[l][0:1408, d * 128:(d + 1) * 128].rearrange("(k p) c -> p k c", p=128)], 11)
                    (wd1,) = wload([w_down[l][1408:2816, d * 128:(d + 1) * 128].rearrange("(k p) c -> p k c", p=128)], 11)
                    for gi, (t0, n) in enumerate(blk):
                        pd = nps((0, 1, 2))
                        for kf in range(NFC):
                            wsl = wd0[:, kf, :] if kf < 11 else wd1[:, kf - 11, :]
                            P.mm(pd[:, 0:n], wsl, G[:, kf, t0 - b0:t0 - b0 + n], start=(kf == 0), stop=(kf == NFC - 1))
                        P.act(YF2[cnt % 2][:, 0:n], pd[:, 0:n], AF.Copy, scale=1.0)
                        P.copy("dve", H2[:, d, t0 - b0:t0 - b0 + n], YF2[cnt % 2][:, 0:n])
                        P.tt("pool", SQT[cnt % 2][:, 0:n], YF2[cnt % 2][:, 0:n], YF2[cnt % 2][:, 0:n], ALU.mult)
                        P.mm(stat[gi][:, 0:n], onesb, SQT[cnt % 2][:, 0:n], start=(d == 0), stop=(d == 7))
                        cnt += 1
                if stop == 'f3':
                    return
                for gi, (t0, n) in enumerate(blk):
                    w = 1 if t0 < TCX else 0
                    rstd_from(stat[gi][:, 0:n], RS3[gi][:, 0:n], 1.0 / D)
                    for d in range(8):
                        P.tt("dve", TM3[d % 2][:, 0:n], H2[:, d, t0 - b0:t0 - b0 + n], RS3[gi][:, 0:n], ALU.mult)
                        P.stt(X[:, d, t0:t0 + n], TM3[d % 2][:, 0:n], der[:, l * 4 + 3, d, w:w + 1], X[:, d, t0:t0 + n],
                              ALU.mult, ALU.add)

        for l in range(depth):
            layer(l)
        for k in range(8):
            P.store(outT[k * 128:(k + 1) * 128, :], X[:, k, :], sem_o)
        P.finalize()
    return nc


def _host_layout(inputs):
    f = lambda a: np.ascontiguousarray(np.asarray(a, dtype=np.float32))
    x, c, ctx, c_ctx = f(inputs["x"]), f(inputs["c"]), f(inputs["ctx"]), f(inputs["c_ctx"])
    B = x.shape[0]
    vecs = np.zeros((128, NVEC), np.float32)
    ng = f(inputs["norm_g"]).reshape(DEPTH, 4, 8, 128)
    vecs[:, V_NORMG:V_NORMG + 128] = ng.transpose(3, 0, 1, 2).reshape(128, -1)
    ba = f(inputs["b_ada"]).reshape(DEPTH, 48, 128)
    vecs[:, V_BADA:V_BADA + 192] = ba.transpose(2, 0, 1).reshape(128, -1)
    cw = f(inputs["conv_w"]).reshape(DEPTH, 31, 4, 128)
    vecs[:, V_CONVW:V_CONVW + 496] = cw.transpose(3, 0, 2, 1).reshape(128, -1)
    for name, off in (("conv_b", V_CONVB), ("conv_ln_g", V_LNG), ("conv_ln_b", V_LNB)):
        vecs[:, off:off + 16] = f(inputs[name]).reshape(DEPTH, 4, 128).transpose(2, 0, 1).reshape(128, -1)
    fw = f(inputs["ffn_conv_w"]).reshape(DEPTH, 3, NFC, 128)
    vecs[:, V_FFNW:V_FFNW + 264] = fw.transpose(3, 0, 2, 1).reshape(128, -1)
    vecs[:, V_FFNB:V_FFNB + 88] = f(inputs["ffn_conv_b"]).reshape(DEPTH, NFC, 128).transpose(2, 0, 1).reshape(128, -1)
    vecs[:, V_IDENT:V_IDENT + 128] = np.eye(128, dtype=np.float32)
    vecs[:, V_ONES:V_ONES + 128] = 1.0
    s_idx = np.arange(128)[:, None]
    j_idx = np.arange(128)[None, :]
    vecs[:, V_TRIF:V_TRIF + 128] = (s_idx <= j_idx).astype(np.float32)
    vecs[:, V_TRIB:V_TRIB + 128] = (s_idx >= j_idx).astype(np.float32)
    lvecs = np.zeros((DEPTH, 128, NLV), np.float32)
    lvecs[:, :, 0:512] = f(inputs["mlstm_norm_g"])[:, None, :]
    bg = f(inputs["b_gates"]).reshape(DEPTH, 2, 2, 4)
    lvecs[:, :, 512:656] = np.broadcast_to(bg[:, None, None, :, 0, :], (DEPTH, 128, 18, 2, 4)).reshape(DEPTH, 128, 144)
    lvecs[:, :, 656:800] = np.broadcast_to(bg[:, None, None, :, 1, :], (DEPTH, 128, 18, 2, 4)).reshape(DEPTH, 128, 144)
    shared = {"vecs": vecs, "lvecs": lvecs, "w_ada": f(inputs["w_ada"]), "w_in": f(inputs["w_in"]),
              "wproj": f(inputs["w_out"]), "w_up": f(inputs["w_up"]), "w_down": f(inputs["w_down"])}
    maps = []
    for b in range(B):
        m = dict(shared)
        m["xT"] = np.ascontiguousarray(np.concatenate([ctx[b], x[b]], axis=0).T)
        cvv = np.stack([c[b].reshape(8, 128).T, c_ctx.reshape(8, 128).T], axis=-1)
        m["cvec"] = np.ascontiguousarray(cvv.reshape(128, 16))
        maps.append(m)
    return maps


_NC_CACHE = {}
_DBG = {}


def kernel(**inputs):
    maps = _host_layout(inputs)
    if "nc" not in _NC_CACHE:
        _NC_CACHE["nc"] = build(DEPTH)
    res = run_bass_kernel_spmd(_NC_CACHE["nc"], maps, core_ids=list(range(len(maps))))
    out = np.stack([np.ascontiguousarray(r["outT"][:, TCX:].T) for r in res.results], axis=0)
    return out.astype(np.float32)
```

```python
import bisect
from contextlib import ExitStack

import numpy as np
import concourse.bass as bass
import concourse.mybir as mybir
from concourse.bass_utils import run_bass_kernel_spmd

F32 = mybir.dt.float32
BF16 = mybir.dt.bfloat16
AF = mybir.ActivationFunctionType
ALU = mybir.AluOpType
AX = mybir.AxisListType

D = 1024
NT = 2304
TCX = 256
DEPTH = 4
DFF = 2816
NFC = 22
EPS = 1e-6
KSCALE = 128 ** -0.5
_ESZ = {F32: 4, BF16: 2}


def _esz(dt):
    return _ESZ[dt]


def ap_intervals(ap):
    if ap.tensor.name.startswith("ps"):
        return [(0, 2048)]
    esz = _esz(ap.dtype)
    dims = list(ap.ap)
    pstride = dims[0][0]
    off = ap.offset
    free_off = off % pstride if pstride > 0 else off
    fd = [(abs(s), c) for (s, c) in dims[1:] if c > 1 and s != 0]
    if not fd:
        return [(free_off * esz, (free_off + 1) * esz)]
    fd.sort()
    s0, c0 = fd[0]
    span = (c0 - 1) * s0 + 1
    runs = [free_off]
    for (s, c) in fd[1:]:
        if len(runs) == 1 and s <= span:
            span = (c - 1) * s + span
        else:
            if len(runs) * c > 64:
                lo = runs[0]
                hi = runs[-1] + span + (c - 1) * s
                runs = [lo]
                span = hi - lo
            else:
                runs = [r + i * s for i in range(c) for r in runs]
                runs.sort()
    return [(r * esz, (r + span) * esz) for r in runs]


class _IMap:
    def __init__(self):
        self.starts = [0]
        self.segs = [[None, {}]]

    def _split(self, pos):
        i = bisect.bisect_right(self.starts, pos) - 1
        if self.starts[i] == pos:
            return i
        w, r = self.segs[i]
        self.starts.insert(i + 1, pos)
        self.segs.insert(i + 1, [w, dict(r)])
        return i + 1

    def rng(self, lo, hi):
        i = self._split(lo)
        j = self._split(hi)
        return range(i, j)


class Prog:
    ENGS = ("pe", "act", "dve", "pool", "sp")

    def __init__(self, nc, es):
        self.nc = nc
        self.es = es
        self.sem = {e: es.enter_context(nc.semaphore("s_" + e)) for e in ("pe", "act", "dve", "pool")}
        self.tick = {e: 0 for e in self.ENGS}
        self.ops = {e: [] for e in self.ENGS}
        self.seen = {e: {} for e in self.ENGS}
        self.dsem = []
        self.maps = {}
        self.nops = 0

    def dma_slot(self):
        s = self.es.enter_context(self.nc.semaphore("d%d" % len(self.dsem)))
        self.dsem.append([s, 0])
        return len(self.dsem) - 1

    def _segs(self, ap):
        m = self.maps.get(ap.tensor.name)
        if m is None:
            m = self.maps[ap.tensor.name] = _IMap()
        for lo, hi in ap_intervals(ap):
            for i in m.rng(lo, hi):
                yield m.segs[i]

    def emit(self, eng, fn, reads, writes, sig=True, dma=None):
        deps = {}

        def add(sv):
            if sv is None:
                return
            s, v = sv
            if s[0] == "d":
                v = self.dsem[s[1]][1]
            if deps.get(s, 0) < v:
                deps[s] = v

        rsegs = [sg for ap in reads for sg in self._segs(ap)]
        wsegs = [sg for ap in writes for sg in self._segs(ap)]
        for sg in rsegs:
            add(sg[0])
        for sg in wsegs:
            add(sg[0])
            for s, v in sg[1].items():
                add((s, v))
        if dma is not None:
            self.dsem[dma][1] += 16
            me = (("d", dma), self.dsem[dma][1])
        elif sig:
            self.tick[eng] += 1
            me = (("e", eng), self.tick[eng])
        else:
            me = (("e", eng), self.tick[eng] + 1)
        waits = []
        for s, v in deps.items():
            if s == ("e", "pe") and eng == "pe":
                continue
            if s == ("e", "pool") and eng == "pool":
                continue
            if self.seen[eng].get(s, 0) >= v:
                continue
            self.seen[eng][s] = v
            waits.append((s, v))
        for sg in rsegs:
            if sg[1].get(me[0], 0) < me[1]:
                sg[1][me[0]] = me[1]
        for sg in wsegs:
            sg[0] = me
            sg[1] = {}
        self.ops[eng].append((fn, waits, (dma if dma is not None else (eng if sig else None))))
        self.nops += 1

    def mm(self, out, lhsT, rhs, start=True, stop=True):
        assert int(np.prod(out.shape[1:])) == int(np.prod(rhs.shape[1:])), (out.shape, rhs.shape)
        assert out.shape[0] == int(np.prod(lhsT.shape[1:])), (out.shape, lhsT.shape)
        assert lhsT.shape[0] == rhs.shape[0], (lhsT.shape, rhs.shape)
        self.emit("pe", lambda e: e.matmul(out, lhsT, rhs, start=start, stop=stop), [lhsT, rhs], [out], sig=stop)

    def tr(self, out, in_, ident):
        self.emit("pe", lambda e: e.transpose(out, in_, ident), [in_, ident], [out], sig=True)

    def act(self, out, in_, func, bias=None, scale=None):
        kw = {}
        rd = [in_]
        if bias is not None:
            kw["bias"] = bias
            if not isinstance(bias, (int, float)):
                rd.append(bias)
        if scale is not None:
            kw["scale"] = scale
            if not isinstance(scale, (int, float)):
                rd.append(scale)
        self.emit("act", lambda e: e.activation(out, in_, func, **kw), rd, [out])

    def tt(self, eng, out, in0, in1, op):
        self.emit(eng, lambda e: e.tensor_tensor(out, in0, in1, op), [in0, in1], [out])

    def ts(self, eng, out, in0, s1, op0, s2=None, op1=None):
        rd = [in0]
        if not isinstance(s1, (int, float)):
            rd.append(s1)
        if s2 is not None and not isinstance(s2, (int, float)):
            rd.append(s2)
        if op1 is None:
            self.emit(eng, lambda e: e.tensor_scalar(out, in0, s1, None, op0), rd, [out])
        else:
            self.emit(eng, lambda e: e.tensor_scalar(out, in0, s1, s2, op0, op1), rd, [out])

    def stt(self, out, in0, sc, in1, op0, op1):
        rd = [in0, in1]
        if not isinstance(sc, (int, float)):
            rd.append(sc)
        self.emit("dve", lambda e: e.scalar_tensor_tensor(out, in0, sc, in1, op0, op1), rd, [out])

    def copy(self, eng, out, in_):
        if eng == "act":
            self.emit("act", lambda e: e.copy(out, in_), [in_], [out])
        else:
            self.emit(eng, lambda e: e.tensor_copy(out, in_), [in_], [out])

    def memset(self, eng, out, val):
        self.emit(eng, lambda e: e.memset(out, val), [], [out])

    def recip(self, out, in_):
        self.emit("dve", lambda e: e.reciprocal(out, in_), [in_], [out])

    def load(self, out, in_, slot, q="sp"):
        self.emit(q, lambda e: e.dma_start(out=out, in_=in_), [], [out], dma=slot)

    def store(self, out, in_, slot, q="sp"):
        self.emit(q, lambda e: e.dma_start(out=out, in_=in_), [in_], [], dma=slot)

    def finalize(self):
        nc = self.nc
        fin = []
        for e in ("pe", "act", "dve", "pool"):
            if self.tick[e] > 0:
                fin.append((("e", e), self.tick[e]))
        for i, (s, c) in enumerate(self.dsem):
            if c > 0:
                fin.append((("d", i), c))
        self.ops["sp"].append((None, fin, None))

        def semof(s):
            return self.sem[s[1]] if s[0] == "e" else self.dsem[s[1]][0]

        def run(e, lst):
            for fn, waits, inc in lst:
                for s, v in waits:
                    e.wait_ge(semof(s), v)
                if fn is None:
                    continue
                ins = fn(e)
                if inc is None:
                    continue
                if isinstance(inc, int):
                    ins.then_inc(self.dsem[inc][0], 16)
                else:
                    ins.then_inc(self.sem[inc], 1)

        with nc.Block() as block:
            @block.tensor
            def _(e):
                run(e, self.ops["pe"])

            @block.scalar
            def _(e):
                run(e, self.ops["act"])

            @block.vector
            def _(e):
                run(e, self.ops["dve"])

            @block.gpsimd
            def _(e):
                run(e, self.ops["pool"])

            @block.sync
            def _(e):
                run(e, self.ops["sp"])


class Arena:
    def __init__(self, nc, es, name, nbytes):
        self.t = es.enter_context(nc.sbuf_tensor(name, [128, nbytes // 4], F32))
        self.ap = self.t[:, :] if not hasattr(self.t, "ap") else self.t.ap()
        self.cap = nbytes
        self.top = 0

    def at(self, off, shape, dt):
        n = int(np.prod(shape))
        nb = n * _esz(dt)
        assert off % 4 == 0 and off + nb <= self.cap, (off, nb, self.cap)
        a = self.ap[:, off // 4:(off + nb + 3) // 4]
        if dt != F32:
            a = a.bitcast(dt)[:, 0:n]
        if len(shape) == 2:
            a = a.rearrange("p (a b) -> p a b", b=shape[1])
        elif len(shape) == 3:
            a = a.rearrange("p (a b c) -> p a b c", b=shape[1], c=shape[2])
        elif len(shape) == 4:
            a = a.rearrange("p (a b c d) -> p a b c d", b=shape[1], c=shape[2], d=shape[3])
        return a

    def alloc(self, shape, dt):
        off = (self.top + 31) // 32 * 32
        n = int(np.prod(shape)) * _esz(dt)
        self.top = off + (n + 3) // 4 * 4
        assert self.top <= self.cap, ("arena overflow", self.top, self.cap)
        return self.at(off, shape, dt)


class Sub:
    def __init__(self, A, base, size):
        self.A, self.base, self.size, self.top = A, base, size, 0

    def reset(self):
        self.top = 0

    def alloc(self, shape, dt):
        off = (self.top + 31) // 32 * 32
        n = int(np.prod(shape)) * _esz(dt)
        self.top = off + (n + 3) // 4 * 4
        assert self.top <= self.size, ("sub overflow", self.top, self.size)
        return self.A.at(self.base + off, shape, dt)


V_NORMG, V_BADA, V_CONVW, V_CONVB, V_LNG, V_LNB, V_FFNW, V_FFNB = 0, 128, 320, 816, 832, 848, 864, 1128
V_IDENT, V_ONES, V_TRIF, V_TRIB, NVEC = 1216, 1344, 1472, 1600, 1728
NLV = 800
SUBG = [(i * 256, 256) for i in range(9)]
GROUPS = [(0, 256)] + [(256 + 512 * i, 512) for i in range(4)]
FBLOCKS = [[(0, 256), (256, 512), (768, 384)], [(1152, 512), (1664, 512), (2176, 128)]]
ORDER = [list(range(18)), [1, 0] + list(range(17, 1, -1))]


_DBG = {}


def build(depth=DEPTH, stop=None):
    nc = bass.Bass("TRN2", target_bir_lowering=False)
    xT = nc.dram_tensor("xT", [D, NT], F32, kind="ExternalInput").ap()
    cvec_d = nc.dram_tensor("cvec", [128, 16], F32, kind="ExternalInput").ap()
    vecs_d = nc.dram_tensor("vecs", [128, NVEC], F32, kind="ExternalInput").ap()
    lvecs_d = nc.dram_tensor("lvecs", [DEPTH, 128, NLV], F32, kind="ExternalInput").ap()
    w_ada = nc.dram_tensor("w_ada", [DEPTH, D, 6 * D], F32, kind="ExternalInput").ap()
    w_in = nc.dram_tensor("w_in", [DEPTH, D, 3088], F32, kind="ExternalInput").ap()
    w_out = nc.dram_tensor("wproj", [DEPTH, D, D], F32, kind="ExternalInput").ap()
    w_up = nc.dram_tensor("w_up", [DEPTH, D, 2 * DFF], F32, kind="ExternalInput").ap()
    w_down = nc.dram_tensor("w_down", [DEPTH, DFF, D], F32, kind="ExternalInput").ap()
    outT = nc.dram_tensor("outT", [D, NT], F32, kind="ExternalOutput").ap()

    with ExitStack() as es:
        P = Prog(nc, es)
        A = Arena(nc, es, "arena", 211968)
        psl = [es.enter_context(nc.psum_tensor("ps%d" % i, [128, 512], F32)) for i in range(6)]
        ps = [p[:, :] for p in psl]
        psbs = [es.enter_context(nc.psum_tensor("psb%d" % i, [128, 1024], BF16))[:, :] for i in range(2)]
        pctr = [0]

        rot = [0, 1, 2, 3, 4, 5]

        def nps(lst=None):
            lst = rot if lst is None else lst
            pctr[0] += 1
            return ps[lst[pctr[0] % len(lst)]]

        bctr = [0]

        def npsb():
            bctr[0] += 1
            i = bctr[0] % 2
            return psbs[i][:, 0:128]

        X = A.alloc([8, NT], F32)
        V = A.alloc([NVEC], F32)
        LV = A.alloc([NLV], F32)
        CB = A.alloc([4, 128], BF16)
        identb, onesb, trifb, tribb = CB[:, 0, :], CB[:, 1, :], CB[:, 2, :], CB[:, 3, :]
        maskb = [trifb, tribb]
        identf, onesf = V[:, V_IDENT:V_IDENT + 128], V[:, V_ONES:V_ONES + 128]
        triff, tribf = V[:, V_TRIF:V_TRIF + 128], V[:, V_TRIB:V_TRIB + 128]
        cst = A.alloc([4], F32)
        c_eps, c_one = cst[:, 0:1], cst[:, 1:2]
        cv = A.alloc([8, 2], F32)
        sv = A.alloc([8, 2], F32)
        modv = A.alloc([DEPTH, 48, 2], F32)
        der = A.alloc([DEPTH * 4, 8, 2], F32)
        normg = V[:, V_NORMG:V_NORMG + 128].rearrange("p (l n k) -> p l n k", n=4, k=8)
        bada = V[:, V_BADA:V_BADA + 192].rearrange("p (l f) -> p l f", f=48)
        convw = V[:, V_CONVW:V_CONVW + 496].rearrange("p (l c k) -> p l c k", c=4, k=31)
        convb = V[:, V_CONVB:V_CONVB + 16].rearrange("p (l c) -> p l c", c=4)
        lng = V[:, V_LNG:V_LNG + 16].rearrange("p (l c) -> p l c", c=4)
        lnb = V[:, V_LNB:V_LNB + 16].rearrange("p (l c) -> p l c", c=4)
        ffnw = V[:, V_FFNW:V_FFNW + 264].rearrange("p (l f k) -> p l f k", f=22, k=3)
        ffnb = V[:, V_FFNB:V_FFNB + 88].rearrange("p (l f) -> p l f", f=22)
        mng = LV[:, 0:512]
        bgi = LV[:, 512:656].rearrange("p (t d h) -> p t d h", d=2, h=4)
        bgf = LV[:, 656:800].rearrange("p (t d h) -> p t d h", d=2, h=4)
        WST = [A.alloc([2048], F32) for _ in range(2)]
        WBF = [A.alloc([2048], BF16) for _ in range(4)]
        wst_sem = [P.dma_slot() for _ in range(2)]
        wctr = [0, 0]
        H_off = (A.top + 31) // 32 * 32
        H = A.alloc([8, NT], BF16)
        QM_off = (A.top + 31) // 32 * 32
        QM = A.alloc([4, NT], BF16)
        S_off = (A.top + 31) // 32 * 32
        S = Sub(A, S_off, A.cap - S_off)
        HS = Sub(A, H_off + 8 * 1152 * 2, 8 * 1152 * 2)
        G = A.at(QM_off, [NFC, 1152], BF16)
        H2 = A.at(H_off, [8, 1152], BF16)
        sem_x, sem_v, sem_lv, sem_o = P.dma_slot(), P.dma_slot(), P.dma_slot(), P.dma_slot()
        _DBG.update(H_off=H_off, QM_off=QM_off, S_off=S_off)

        def wload(pieces, nk):
            si = wctr[0] % 2
            wctr[0] += 1
            bi = wctr[1] % 4
            wctr[1] += 1
            off = 0
            outs = []
            for pc in pieces:
                ncol = pc.shape[2]
                n = nk * ncol
                stv = WST[si][:, off:off + n].rearrange("p (k c) -> p k c", c=ncol)
                P.load(stv, pc, wst_sem[si])
                outs.append(WBF[bi][:, off:off + n].rearrange("p (k c) -> p k c", c=ncol))
                off += n
            assert off <= 2048
            P.copy("pool", WBF[bi][:, 0:off], WST[si][:, 0:off])
            return outs

        def wcols(w3, l, c0, ncol):
            return w3[l].rearrange("(k p) c -> p k c", p=128)[:, :, c0:c0 + ncol]

        for k in range(8):
            P.load(X[:, k, :], xT[k * 128:(k + 1) * 128, :], sem_x)
        P.load(V, vecs_d, sem_v)
        P.load(cv, cvec_d.rearrange("p (k w) -> p k w", w=2), sem_v)
        P.memset("dve", cst[:, 0:1], EPS)
        P.memset("dve", cst[:, 1:2], 1.0)
        P.copy("dve", CB[:, 0, :], identf)
        P.copy("dve", CB[:, 1, :], onesf)
        P.copy("dve", CB[:, 2, :], triff)
        P.copy("dve", CB[:, 3, :], tribf)
        P.act(sv, cv, AF.Silu)
        def adaln_gen(l, pm):
            for j in range(24):
                si = wctr[0] % 2
                wctr[0] += 1
                stv = WST[si].rearrange("p (k c) -> p k c", c=256)
                P.load(stv, wcols(w_ada, l, j * 256, 256), wst_sem[si])
                for fc in range(2):
                    f = j * 2 + fc
                    for k in range(8):
                        P.mm(pm[:, 2 * f:2 * f + 2], stv[:, k, fc * 128:(fc + 1) * 128], sv[:, k, :],
                             start=(k == 0), stop=(k == 7))
                yield j

        def adaln_finish(l, pm):
            pm3 = pm[:, 0:96].rearrange("p (f w) -> p f w", w=2)
            for w in range(2):
                P.tt("dve", modv[:, l, :, w], pm3[:, :, w], bada[:, l, :], ALU.add)
            for w in range(2):
                P.stt(der[:, l * 4 + 0, :, w], modv[:, l, 8:16, w], 1.0, normg[:, l, 0, :], ALU.add, ALU.mult)
                P.tt("dve", der[:, l * 4 + 1, :, w], modv[:, l, 16:24, w], normg[:, l, 1, :], ALU.mult)
                P.stt(der[:, l * 4 + 2, :, w], modv[:, l, 32:40, w], 1.0, normg[:, l, 2, :], ALU.add, ALU.mult)
                P.tt("dve", der[:, l * 4 + 3, :, w], modv[:, l, 40:48, w], normg[:, l, 3, :], ALU.mult)

        for _ in adaln_gen(0, ps[5]):
            pass
        adaln_finish(0, ps[5])

        def split3(src, spl, spr, n, rows_=128):
            h = [spl[0:rows_, j, 0:n] for j in range(3)]
            r = spr[0:rows_, 0:n]
            P.copy("dve", h[0], src)
            P.tt("dve", r, src, h[0], ALU.subtract)
            P.copy("dve", h[1], r)
            P.tt("dve", r, r, h[1], ALU.subtract)
            P.copy("dve", h[2], r)
            return h

        def rstd_from(psum_ap, out_ap, inv_n):
            P.act(out_ap, psum_ap, AF.Ln, bias=c_eps, scale=inv_n)
            P.act(out_ap, out_ap, AF.Exp, scale=-0.5)

        def normmod(l, subs, kind_gs, sh_base, dst, sub):
            sq = [sub.alloc([8, 256], BF16) for _ in range(2)]
            rsb = [sub.alloc([256], F32) for _ in range(2)]
            tmp = [sub.alloc([256], F32) for _ in range(3)]
            for i, (t0, n) in enumerate(subs):
                w = 1 if t0 < TCX else 0
                pp = nps()
                for k in range(8):
                    P.tt("pool", sq[i % 2][:, k, 0:n], X[:, k, t0:t0 + n], X[:, k, t0:t0 + n], ALU.mult)
                for k in range(8):
                    P.mm(pp[:, 0:n], onesb, sq[i % 2][:, k, 0:n], start=(k == 0), stop=(k == 7))
                rstd_from(pp[:, 0:n], rsb[i % 2][:, 0:n], 1.0 / D)
                for k in range(8):
                    tt_ = tmp[k % 3][:, 0:n]
                    P.tt("dve", tt_, X[:, k, t0:t0 + n], rsb[i % 2][:, 0:n], ALU.mult)
                    P.act(dst(k, t0, n), tt_, AF.Identity, bias=modv[:, l, sh_base + k, w:w + 1],
                          scale=der[:, l * 4 + kind_gs, k, w:w + 1])

        def layer(l):
            P.load(LV, lvecs_d[l], sem_lv)
            if stop == 'setup':
                return
            S.reset()
            normmod(l, SUBG, 0, 0, lambda k, t0, n: H[:, k, t0:t0 + n], S)
            if stop == 'norm1':
                return
            S.reset()
            Kb = S.alloc([NT], BF16)
            VTM = S.alloc([18, 130], BF16)
            hs_off = S.base + (S.top + 31) // 32 * 32
            HSUM = S.alloc([18, 128], F32)
            GI = S.alloc([18, 2, 4], F32)
            LF = S.alloc([18, 2, 4], F32)
            NB = S.alloc([18, 2, 4], F32)
            DD = S.alloc([18, 2, 4], F32)
            rows = S.alloc([4, 144], F32)
            MB = S.alloc([288], F32)
            T3 = S.alloc([3, 144], F32)
            SPL = A.at(hs_off, [3, 288], BF16)
            SPR = A.at(hs_off + 2048, [288], F32)
            E3 = S.alloc([3, 144], F32)
            UG = E3[:, 0, :].rearrange("p (t d h) -> p t d h", d=2, h=4)
            WB = E3[:, 1, :].rearrange("p (t d h) -> p t d h", d=2, h=4)
            CL = E3[:, 2, :].rearrange("p (t d h) -> p t d h", d=2, h=4)
            Cst = [S.alloc([129], F32) for _ in range(2)]
            Csb = [[S.alloc([130], BF16) for _ in range(2)] for _ in range(2)]
            PT = [[S.alloc([128], BF16) for _ in range(2)] for _ in range(2)]
            KU = [[S.alloc([128], BF16) for _ in range(2)] for _ in range(2)]
            dn = S.alloc([8], F32)
            hdt = [S.alloc([128], F32) for _ in range(2)]
            stt_ = S.alloc([18, 6], F32)
            mv = S.alloc([18, 2], F32)
            rsd = S.alloc([18], F32)
            OG = [S.alloc([128], F32) for _ in range(2)]
            XN = hdt
            MT = [S.alloc([128], BF16) for _ in range(2)]

            (wg,) = wload([wcols(w_in, l, 3072, 16)], 8)
            pg = nps()
            for t in range(18):
                for k in range(8):
                    P.mm(pg[:, t * 16:(t + 1) * 16], H[:, k, t * 128:(t + 1) * 128], wg[:, k, :],
                         start=(k == 0), stop=(k == 7))
            pg5 = pg[:, 0:288].rearrange("p (t d g h) -> p t d g h", d=2, g=2, h=4)
            P.tt("dve", GI, pg5[:, :, :, 0, :], bgi, ALU.add)
            P.tt("dve", LF, pg5[:, :, :, 1, :], bgf, ALU.add)
            if stop == 'g1':
                return
            LFf = LF.rearrange("p t d h -> p (t d h)")
            GIf = GI.rearrange("p t d h -> p (t d h)")
            NBf = NB.rearrange("p t d h -> p (t d h)")
            DDf = DD.rearrange("p t d h -> p (t d h)")
            P.ts("dve", LFf, LFf, -60.0, ALU.max)
            P.act(LFf, LFf, AF.Exp, scale=-1.0)
            P.act(LFf, LFf, AF.Ln, bias=c_one, scale=1.0)
            if stop == 'g2a':
                return
            pc_ = nps()
            L3 = split3(LFf, SPL, SPR, 144)
            for j3, hb in enumerate(L3):
                P.mm(pc_[:, 0:144], trifb, hb, start=(j3 == 0), stop=(j3 == 2))
            pc2 = nps()
            for j3, hb in enumerate(L3):
                P.mm(pc2[:, 0:144], tribb, hb, start=(j3 == 0), stop=(j3 == 2))
            pc3 = nps()
            for j3, hb in enumerate(L3):
                P.mm(pc3[:, 0:144], onesb, hb, start=(j3 == 0), stop=(j3 == 2))
            pcf = pc_[:, 0:144].rearrange("p (t d h) -> p t d h", d=2, h=4)
            pcb = pc2[:, 0:144].rearrange("p (t d h) -> p t d h", d=2, h=4)
            P.copy("act", NB[:, :, 0, :], pcf[:, :, 0, :])
            P.copy("act", NB[:, :, 1, :], pcb[:, :, 1, :])
            P.tt("dve", DDf, GIf, NBf, ALU.add)
            if stop == 'g2':
                return
            P.emit("pool", lambda e: e.tensor_reduce(rows[0:1, 0, :], DDf, AX.C, ALU.max), [DDf], [rows[0:1, 0, :]])
            P.copy("act", rows[0:1, 1, :], pc3[0:1, 0:144])
            P.memset("dve", rows[0:1, 3, :], 0.0)
            if stop == 'g3':
                return
            r4 = rows[0:1, :, :].rearrange("p r (t d h) -> p r t d h", d=2, h=4)
            for dr in range(2):
                od = ORDER[dr]
                for i, t in enumerate(od):
                    P.tt("dve", r4[:, 2, t, dr, :], r4[:, 3, t, dr, :], r4[:, 0, t, dr, :], ALU.max)
                    if i + 1 < 18:
                        P.tt("dve", r4[:, 3, od[i + 1], dr, :], r4[:, 2, t, dr, :], r4[:, 1, t, dr, :], ALU.subtract)
            if stop == 'g4':
                return
            pmb = nps()
            R3 = split3(rows[0:1, 2:4, :].rearrange("p r n -> p (r n)"), SPL, SPR, 288, rows_=1)
            for j3, hb in enumerate(R3):
                P.mm(pmb[:, 0:288], onesb[0:1, :], hb, start=(j3 == 0), stop=(j3 == 2))
            P.copy("act", MB, pmb[:, 0:288])
            P.tt("dve", T3[:, 0, :], DDf, MB[:, 0:144], ALU.subtract)
            P.tt("dve", T3[:, 1, :], MB[:, 144:288], MB[:, 0:144], ALU.subtract)
            P.tt("dve", T3[:, 2, :], NBf, MB[:, 0:144], ALU.subtract)
            P.ts("dve", T3[:, 2, :], T3[:, 2, :], 80.0, ALU.min)
            P.act(E3.rearrange("p a b -> p (a b)"), T3.rearrange("p a b -> p (a b)"), AF.Exp)
            P.memset("pool", VTM[:, :, 128:130], 1.0)

            if stop == 'gates':
                return
            ag = None
            if l + 1 < depth:
                rot[:] = [0, 1, 2, 3, 4]
                ag = adaln_gen(l + 1, ps[5])
            for hd in range(4):
                wq, wk = wload([wcols(w_in, l, 1024 + hd * 128, 128), wcols(w_in, l, 1536 + hd * 128, 128)], 8)
                wv, wo = wload([wcols(w_in, l, 2048 + hd * 128, 128), wcols(w_in, l, 2560 + hd * 128, 128)], 8)
                for (t0, n) in GROUPS:
                    pq = nps()
                    for k in range(8):
                        P.mm(pq[:, 0:n], wq[:, k, :], H[:, k, t0:t0 + n], start=(k == 0), stop=(k == 7))
                    P.act(QM[:, hd, t0:t0 + n], pq[:, 0:n], AF.Copy, scale=KSCALE)
                    pk = nps()
                    for k in range(8):
                        P.mm(pk[:, 0:n], wk[:, k, :], H[:, k, t0:t0 + n], start=(k == 0), stop=(k == 7))
                    P.copy("dve", Kb[:, t0:t0 + n], pk[:, 0:n])
                for t4 in range(0, 18, 4):
                    nt_ = min(4, 18 - t4)
                    pv = nps()
                    for i in range(nt_):
                        t = t4 + i
                        for k in range(8):
                            P.mm(pv[:, i * 128:(i + 1) * 128], H[:, k, t * 128:(t + 1) * 128], wv[:, k, :],
                                 start=(k == 0), stop=(k == 7))
                    P.copy("act", VTM[:, t4:t4 + nt_, 0:128],
                           pv[:, 0:nt_ * 128].rearrange("p (a b) -> p a b", b=128))
                P.memset("pool", HSUM, 0.0)
                for dr in range(2):
                    P.memset("pool", Cst[dr], 0.0)
                for step in range(18):
                    for dr in range(2):
                        t = ORDER[dr][step]
                        par = step % 2
                        tk = slice(t * 128, (t + 1) * 128)
                        u_ap = UG[:, t, dr, hd:hd + 1]
                        w_ap = WB[:, t, dr, hd:hd + 1]
                        pS = nps()
                        P.mm(pS[:, 0:128], Kb[:, tk], QM[:, hd, tk])
                        pT = npsb()
                        P.tr(pT, Kb[:, tk], identb)
                        P.stt(PT[dr][par], pS[:, 0:128], u_ap, maskb[dr], ALU.mult, ALU.mult)
                        P.act(KU[dr][par], pT, AF.Copy, scale=u_ap)
                        P.ts("pool", Csb[dr][par][:, 0:129], Cst[dr], w_ap, ALU.mult)
                        pN = nps()
                        P.mm(pN[:, 0:129], PT[dr][par], VTM[:, t, 0:129], start=True, stop=False)
                        P.mm(pN[:, 0:129], QM[:, hd, tk], Csb[dr][par][:, 0:129], start=False, stop=True)
                        pC = nps()
                        P.mm(pC[:, 0:129], KU[dr][par], VTM[:, t, 0:129])
                        P.stt(Cst[dr], Cst[dr], w_ap, pC[:, 0:129], ALU.mult, ALU.add)
                        dcol = dn[:, dr * 2:dr * 2 + 1]
                        rcol = dn[:, dr * 2 + 1:dr * 2 + 2]
                        P.ts("dve", dcol, pN[:, 128:129], -1.0, ALU.mult, CL[:, t, dr, hd:hd + 1], ALU.max)
                        P.ts("dve", dcol, pN[:, 128:129], dcol, ALU.max)
                        P.recip(rcol, dcol)
                        P.act(hdt[dr], pN[:, 0:128], AF.Copy, scale=rcol)
                        P.tt("pool", HSUM[:, t, :], HSUM[:, t, :], hdt[dr], ALU.add)
                    if ag is not None and step % 3 == 2:
                        next(ag, None)
                for t in range(18):
                    P.emit("dve", lambda e, t=t: e.bn_stats(stt_[:, t, :], HSUM[:, t, :]), [HSUM[:, t, :]], [stt_[:, t, :]])
                    P.emit("dve", lambda e, t=t: e.bn_aggr(mv[:, t, :], stt_[:, t, :]), [stt_[:, t, :]], [mv[:, t, :]])
                P.act(rsd, mv[:, :, 1], AF.Ln, bias=c_eps, scale=1.0)
                P.act(rsd, rsd, AF.Exp, scale=-0.5)
                for t4 in range(0, 18, 4):
                    nt_ = min(4, 18 - t4)
                    pbs = []
                    for i in range(nt_):
                        t = t4 + i
                        po = nps()
                        for k in range(8):
                            P.mm(po[:, 0:128], H[:, k, t * 128:(t + 1) * 128], wo[:, k, :], start=(k == 0), stop=(k == 7))
                        P.act(OG[t % 2], po[:, 0:128], AF.Sigmoid)
                        P.tt("pool", OG[t % 2], OG[t % 2], mng[:, hd * 128:(hd + 1) * 128], ALU.mult)
                        P.ts("dve", XN[t % 2], HSUM[:, t, :], mv[:, t, 0:1], ALU.subtract, rsd[:, t:t + 1], ALU.mult)
                        P.tt("dve", MT[t % 2], XN[t % 2], OG[t % 2], ALU.mult)
                        pb = npsb()
                        P.tr(pb, MT[t % 2], identb)
                        P.copy("act", QM[:, hd, t * 128:(t + 1) * 128], pb)

            if ag is not None:
                for _ in ag:
                    pass
                adaln_finish(l + 1, ps[5])
                rot[:] = [0, 1, 2, 3, 4, 5]
            if stop == 'mlstm':
                return
            S.reset()
            UB = S.alloc([4, NT], BF16)
            s_mark = S.top
            UP = S.alloc([2880], BF16)
            SG = [S.alloc([512], F32) for _ in range(2)]
            P.memset("pool", UP, 0.0)

            def up_view(t0, n, shift):
                if t0 < TCX:
                    return UP[:, 16 + shift:16 + shift + 256]
                r0 = (t0 - TCX) // 64
                base = 288 + 80 * r0 + shift
                rws = n // 64
                return UP[:, base:base + 80 * rws].rearrange("p (r c) -> p r c", c=80)[:, :, 0:64]

            def rows_view(ap2, t0, n):
                if t0 < TCX:
                    return ap2
                return ap2.rearrange("p (r c) -> p r c", c=64)

            for c in range(4):
                wa, wgl = wload([wcols(w_in, l, c * 128, 128), wcols(w_in, l, 512 + c * 128, 128)], 8)
                if c == 0:
                    DBt = S.alloc([31, 128], BF16)
                for kk in range(31):
                    P.ts("pool" if kk % 2 else "dve", DBt[:, kk, :], identb, convw[:, l, c, kk:kk + 1], ALU.mult)
                for gi, (t0, n) in enumerate(GROUPS):
                    pa = nps()
                    for k in range(8):
                        P.mm(pa[:, 0:n], wa[:, k, :], H[:, k, t0:t0 + n], start=(k == 0), stop=(k == 7))
                    pgl = nps()
                    for k in range(8):
                        P.mm(pgl[:, 0:n], wgl[:, k, :], H[:, k, t0:t0 + n], start=(k == 0), stop=(k == 7))
                    P.act(SG[gi % 2][:, 0:n], pgl[:, 0:n], AF.Sigmoid)
                    P.tt("dve", up_view(t0, n, 0), rows_view(pa[:, 0:n], t0, n), rows_view(SG[gi % 2][:, 0:n], t0, n), ALU.mult)
                    py = nps()
                    for kk in range(31):
                        P.mm(rows_view(py[:, 0:n], t0, n), DBt[:, kk, :], up_view(t0, n, kk - 15),
                             start=(kk == 0), stop=(kk == 30))
                    P.act(UB[:, c, t0:t0 + n], py[:, 0:n], AF.Identity, bias=convb[:, l, c:c + 1], scale=1.0)
            if stop == 'c1':
                return
            S.top = s_mark
            SQ = [S.alloc([4, 256], BF16) for _ in range(2)]
            MU = [S.alloc([256], F32) for _ in range(2)]
            RS = [S.alloc([256], F32) for _ in range(2)]
            TM = [S.alloc([256], F32) for _ in range(2)]
            for i, (t0, n) in enumerate(SUBG):
                p1, p2 = nps(), nps()
                for c in range(4):
                    P.act(SQ[i % 2][:, c, :], UB[:, c, t0:t0 + n], AF.Square)
                for c in range(4):
                    P.mm(p1[:, 0:n], onesb, UB[:, c, t0:t0 + n], start=(c == 0), stop=(c == 3))
                for c in range(4):
                    P.mm(p2[:, 0:n], onesb, SQ[i % 2][:, c, :], start=(c == 0), stop=(c == 3))
                mu, rs = MU[i % 2], RS[i % 2]
                P.act(mu, p1[:, 0:n], AF.Copy, scale=1.0 / 512)
                P.tt("dve", rs, mu, mu, ALU.mult)
                P.stt(rs, p2[:, 0:n], 1.0 / 512, rs, ALU.mult, ALU.subtract)
                P.ts("dve", rs, rs, 0.0, ALU.max)
                P.act(rs, rs, AF.Ln, bias=c_eps, scale=1.0)
                P.act(rs, rs, AF.Exp, scale=-0.5)
                for c in range(4):
                    tm = TM[c % 2]
                    P.tt("dve", tm, UB[:, c, t0:t0 + n], mu, ALU.subtract)
                    P.tt("pool", tm, tm, rs, ALU.mult)
                    P.act(UB[:, c, t0:t0 + n], tm, AF.Silu, bias=lnb[:, l, c:c + 1], scale=lng[:, l, c:c + 1])

            if stop == 'conv':
                return
            wo4 = []
            for i in range(4):
                (wv_,) = wload([wcols(w_out, l, i * 256, 256)], 8)
                wo4.append(wv_)
            S.top = s_mark
            YS = [S.alloc([8, 256], BF16) for _ in range(1)]
            YF = S.alloc([8, 256], F32)
            RS2 = [S.alloc([256], F32) for _ in range(2)]
            TM2 = [S.alloc([256], F32) for _ in range(2)]
            for i, (t0, n) in enumerate(SUBG):
                w = 1 if t0 < TCX else 0
                for dh in range(2):
                    for d in range(4 * dh, 4 * dh + 4):
                        yo = ps[d % 4][:, 0:n]
                        for k in range(8):
                            rhs = UB[:, k, t0:t0 + n] if k < 4 else QM[:, k - 4, t0:t0 + n]
                            P.mm(yo, wo4[d // 2][:, k, (d % 2) * 128:(d % 2) * 128 + 128], rhs, start=(k == 0), stop=(k == 7))
                    for d in range(4 * dh, 4 * dh + 4):
                        yo = ps[d % 4][:, 0:n]
                        P.act(YF[:, d, :], yo, AF.Copy, scale=1.0)
                        P.tt("pool", YS[0][:, d, :], YF[:, d, :], YF[:, d, :], ALU.mult)
                pst = ps[4 + i % 2]
                for d in range(8):
                    P.mm(pst[:, 0:n], onesb, YS[0][:, d, :], start=(d == 0), stop=(d == 7))
                if stop == 'w2':
                    continue
                rstd_from(pst[:, 0:n], RS2[i % 2], 1.0 / D)
                for d in range(8):
                    P.tt("dve", TM2[d % 2], YF[:, d, :], RS2[i % 2], ALU.mult)
                    if stop != 'w3':
                        P.stt(X[:, d, t0:t0 + n], TM2[d % 2], der[:, l * 4 + 1, d, w:w + 1], X[:, d, t0:t0 + n], ALU.mult, ALU.add)

            if stop == 'wout':
                return
            for blk in FBLOCKS:
                b0 = blk[0][0]
                HS.reset()
                subs = [(b0 + i * 256, 256) for i in range(1152 // 256)] + [(b0 + 1024, 128)]
                normmod(l, subs, 2, 24, lambda k, t0, n: H2[:, k, t0 - b0:t0 - b0 + n], HS)
                if stop == 'f1':
                    return
                HS.reset()
                GP = [HS.alloc([528], F32) for _ in range(2)]
                ACC = [HS.alloc([512], F32) for _ in range(2)]
                SL = [HS.alloc([512], F32) for _ in range(2)]
                GPC = HS.alloc([264], F32)
                for gp in GP + [GPC]:
                    P.memset("pool", gp, 0.0)

                def gp_view(gp, t0, n, sh):
                    if t0 < TCX:
                        return gp[:, sh:sh + 256]
                    rws = n // 64
                    return gp[:, 0:66 * rws].rearrange("p (r c) -> p r c", c=66)[:, :, sh:sh + 64]

                cnt = 0
                for fp in range(11):
                    (wvl,) = wload([wcols(w_up, l, fp * 256, 256)], 8)
                    (wgt,) = wload([wcols(w_up, l, DFF + fp * 256, 256)], 8)
                    for fc in range(2):
                        f = fp * 2 + fc
                        for (t0, n) in blk:
                            pvv = nps()
                            for k in range(8):
                                P.mm(pvv[:, 0:n], wvl[:, k, fc * 128:(fc + 1) * 128], H2[:, k, t0 - b0:t0 - b0 + n],
                                     start=(k == 0), stop=(k == 7))
                            pgg = nps()
                            for k in range(8):
                                P.mm(pgg[:, 0:n], wgt[:, k, fc * 128:(fc + 1) * 128], H2[:, k, t0 - b0:t0 - b0 + n],
                                     start=(k == 0), stop=(k == 7))
                            gp, acc, sl = (GPC if t0 < TCX else GP[cnt % 2]), ACC[cnt % 2], SL[cnt % 2]
                            cnt += 1
                            P.copy("act", gp_view(gp, t0, n, 1), rows_view(pgg[:, 0:n], t0, n))
                            accv = rows_view(acc[:, 0:n], t0, n)
                            P.ts("dve", accv, gp_view(gp, t0, n, 0), ffnw[:, l, f, 0:1], ALU.mult)
                            P.stt(accv, gp_view(gp, t0, n, 1), ffnw[:, l, f, 1:2], accv, ALU.mult, ALU.add)
                            P.stt(accv, gp_view(gp, t0, n, 2), ffnw[:, l, f, 2:3], accv, ALU.mult, ALU.add)
                            P.act(sl[:, 0:n], acc[:, 0:n], AF.Silu, bias=ffnb[:, l, f:f + 1], scale=1.0)
                            P.tt("dve", G[:, f, t0 - b0:t0 - b0 + n], sl[:, 0:n], pvv[:, 0:n], ALU.mult)
                if stop == 'f2':
                    return
                HS.reset()
                SQT = [HS.alloc([512], BF16) for _ in range(2)]
                YF2 = [HS.alloc([512], F32) for _ in range(2)]
                RS3 = [HS.alloc([512], F32) for _ in range(3)]
                TM3 = [HS.alloc([512], F32) for _ in range(2)]
                stat = [ps[3], ps[4], ps[5]]
                cnt = 0
                for d in range(8):
                    (wd0,) = wload([w_down[l][0:1408, d * 128:(d + 1) * 128].rearrange("(k p) c -> p k c", p=128)], 11)
                    (wd1,) = wload([w_down[l][1408:2816, d * 128:(d + 1) * 128].rearrange("(k p) c -> p k c", p=128)], 11)
                    for gi, (t0, n) in enumerate(blk):
                        pd = nps((0, 1, 2))
                        for kf in range(NFC):
                            wsl = wd0[:, kf, :] if kf < 11 else wd1[:, kf - 11, :]
                            P.mm(pd[:, 0:n], wsl, G[:, kf, t0 - b0:t0 - b0 + n], start=(kf == 0), stop=(kf == NFC - 1))
                        P.act(YF2[cnt % 2][:, 0:n], pd[:, 0:n], AF.Copy, scale=1.0)
                        P.copy("dve", H2[:, d, t0 - b0:t0 - b0 + n], YF2[cnt % 2][:, 0:n])
                        P.tt("pool", SQT[cnt % 2][:, 0:n], YF2[cnt % 2][:, 0:n], YF2[cnt % 2][:, 0:n], ALU.mult)
                        P.mm(stat[gi][:, 0:n], onesb, SQT[cnt % 2][:, 0:n], start=(d == 0), stop=(d == 7))
                        cnt += 1
                if stop == 'f3':
                    return
                for gi, (t0, n) in enumerate(blk):
                    w = 1 if t0 < TCX else 0
                    rstd_from(stat[gi][:, 0:n], RS3[gi][:, 0:n], 1.0 / D)
                    for d in range(8):
                        P.tt("dve", TM3[d % 2][:, 0:n], H2[:, d, t0 - b0:t0 - b0 + n], RS3[gi][:, 0:n], ALU.mult)
                        P.stt(X[:, d, t0:t0 + n], TM3[d % 2][:, 0:n], der[:, l * 4 + 3, d, w:w + 1], X[:, d, t0:t0 + n],
                              ALU.mult, ALU.add)

        for l in range(depth):
            layer(l)
        for k in range(8):
            P.store(outT[k * 128:(k + 1) * 128, :], X[:, k, :], sem_o)
        P.finalize()
    return nc


def _host_layout(inputs):
    f = lambda a: np.ascontiguousarray(np.asarray(a, dtype=np.float32))
    x, c, ctx, c_ctx = f(inputs["x"]), f(inputs["c"]), f(inputs["ctx"]), f(inputs["c_ctx"])
    B = x.shape[0]
    vecs = np.zeros((128, NVEC), np.float32)
    ng = f(inputs["norm_g"]).reshape(DEPTH, 4, 8, 128)
    vecs[:, V_NORMG:V_NORMG + 128] = ng.transpose(3, 0, 1, 2).reshape(128, -1)
    ba = f(inputs["b_ada"]).reshape(DEPTH, 48, 128)
    vecs[:, V_BADA:V_BADA + 192] = ba.transpose(2, 0, 1).reshape(128, -1)
    cw = f(inputs["conv_w"]).reshape(DEPTH, 31, 4, 128)
    vecs[:, V_CONVW:V_CONVW + 496] = cw.transpose(3, 0, 2, 1).reshape(128, -1)
    for name, off in (("conv_b", V_CONVB), ("conv_ln_g", V_LNG), ("conv_ln_b", V_LNB)):
        vecs[:, off:off + 16] = f(inputs[name]).reshape(DEPTH, 4, 128).transpose(2, 0, 1).reshape(128, -1)
    fw = f(inputs["ffn_conv_w"]).reshape(DEPTH, 3, NFC, 128)
    vecs[:, V_FFNW:V_FFNW + 264] = fw.transpose(3, 0, 2, 1).reshape(128, -1)
    vecs[:, V_FFNB:V_FFNB + 88] = f(inputs["ffn_conv_b"]).reshape(DEPTH, NFC, 128).transpose(2, 0, 1).reshape(128, -1)
    vecs[:, V_IDENT:V_IDENT + 128] = np.eye(128, dtype=np.float32)
    vecs[:, V_ONES:V_ONES + 128] = 1.0
    s_idx = np.arange(128)[:, None]
    j_idx = np.arange(128)[None, :]
    vecs[:, V_TRIF:V_TRIF + 128] = (s_idx <= j_idx).astype(np.float32)
    vecs[:, V_TRIB:V_TRIB + 128] = (s_idx >= j_idx).astype(np.float32)
    lvecs = np.zeros((DEPTH, 128, NLV), np.float32)
    lvecs[:, :, 0:512] = f(inputs["mlstm_norm_g"])[:, None, :]
    bg = f(inputs["b_gates"]).reshape(DEPTH, 2, 2, 4)
    lvecs[:, :, 512:656] = np.broadcast_to(bg[:, None, None, :, 0, :], (DEPTH, 128, 18, 2, 4)).reshape(DEPTH, 128, 144)
    lvecs[:, :, 656:800] = np.broadcast_to(bg[:, None, None, :, 1, :], (DEPTH, 128, 18, 2, 4)).reshape(DEPTH, 128, 144)
    shared = {"vecs": vecs, "lvecs": lvecs, "w_ada": f(inputs["w_ada"]), "w_in": f(inputs["w_in"]),
              "wproj": f(inputs["w_out"]), "w_up": f(inputs["w_up"]), "w_down": f(inputs["w_down"])}
    maps = []
    for b in range(B):
        m = dict(shared)
        m["xT"] = np.ascontiguousarray(np.concatenate([ctx[b], x[b]], axis=0).T)
        cvv = np.stack([c[b].reshape(8, 128).T, c_ctx.reshape(8, 128).T], axis=-1)
        m["cvec"] = np.ascontiguousarray(cvv.reshape(128, 16))
        maps.append(m)
    return maps


_NC_CACHE = {}
_DBG = {}


def kernel(**inputs):
    maps = _host_layout(inputs)
    if "nc" not in _NC_CACHE:
        _NC_CACHE["nc"] = build(DEPTH)
    res = run_bass_kernel_spmd(_NC_CACHE["nc"], maps, core_ids=list(range(len(maps))))
    out = np.stack([np.ascontiguousarray(r["outT"][:, TCX:].T) for r in res.results], axis=0)
    return out.astype(np.float32)
```
